# Optimizing a Trainium2 kernel written in Bass

```python
import jax, jax.numpy as jnp
from jax import lax
import numpy as np

D_MODEL = 1024
BATCH = 2
SEQ = 8192
DEPTH = 1

CHUNK = 64
QBLOCK = 128
SB_HEADS = 8
SB_HEAD_DIM = 64
SB_WIDTH = SB_HEADS * SB_HEAD_DIM
HG_HEADS = 8
HG_KDIM = 64
HG_VDIM = 64
HG_WIDTH = HG_HEADS * HG_KDIM
HG_VWIDTH = HG_HEADS * HG_VDIM
N_BRANCHES = 2
N_GROUPS = 4
EXPERTS_PER_GROUP = 4
N_EXPERTS = N_GROUPS * EXPERTS_PER_GROUP
TOP_K_INNER = 2
D_EXPERT = 512
IN_COLS = 3 * SB_WIDTH + 2 * HG_WIDTH + 2 * HG_VWIDTH + N_BRANCHES * D_MODEL
EPS = 1e-6

kernel_name = 'hybrid_stickbreak_hgrn2_hmoe'


def rmsnorm(x, g):
    xf = x.astype(jnp.float32)
    var = jnp.mean(xf * xf, axis=-1, keepdims=True)
    return (xf * lax.rsqrt(var + EPS) * g.astype(jnp.float32)).astype(x.dtype)


def stick_breaking_attention(q, k, v):
    b, h, s, dh = q.shape
    nb = s // QBLOCK
    scale = dh ** -0.5
    key_pos = jnp.arange(s)
    q_blocks = q.reshape(b, h, nb, QBLOCK, dh).transpose(2, 0, 1, 3, 4)

    def one_block(args):
        q_blk, blk = args
        z = jnp.einsum('bhqd,bhkd->bhqk', q_blk, k, preferred_element_type=jnp.float32) * scale
        q_pos = blk * QBLOCK + jnp.arange(QBLOCK)
        mask = key_pos[None, :] < q_pos[:, None]
        log_beta = jax.nn.log_sigmoid(z)
        log_keep = jnp.where(mask, jax.nn.log_sigmoid(-z), 0.0)
        later = jnp.concatenate([log_keep[..., 1:], jnp.zeros_like(log_keep[..., :1])], axis=-1)
        log_survive = lax.cumsum(later, axis=3, reverse=True)
        a = jnp.where(mask, jnp.exp(log_beta + log_survive), 0.0)
        return jnp.einsum('bhqk,bhkd->bhqd', a.astype(v.dtype), v)

    out = lax.map(one_block, (q_blocks, jnp.arange(nb)))
    return out.transpose(1, 2, 0, 3, 4).reshape(b, h, s, dh)


def hgrn2_chunk_scan(q, k, log_f, v):
    b, s, h, dk = q.shape
    dv = v.shape[-1]
    nc = s // CHUNK

    def to_chunks(t):
        return t.reshape(b, nc, CHUNK, h, t.shape[-1]).transpose(1, 0, 3, 2, 4)

    causal = jnp.tril(jnp.ones((CHUNK, CHUNK), dtype=bool))

    def step(state, xs):
        qc, kc, gc, vc = xs
        cum = jnp.cumsum(gc, axis=2)
        diff = cum[:, :, :, None, :] - cum[:, :, None, :, :]
        decay = jnp.exp(jnp.where(causal[:, :, None], diff, -jnp.inf))
        scores = jnp.einsum('bhtk,bhsk,bhtsk->bhts', qc, kc, decay)
        o = (jnp.einsum('bhts,bhsv->bhtv', scores, vc)
             + jnp.einsum('bhtk,bhkv->bhtv', qc * jnp.exp(cum), state))
        last = cum[:, :, -1, :]
        state = (state * jnp.exp(last)[..., None]
                 + jnp.einsum('bhsk,bhsv->bhkv', kc * jnp.exp(last[:, :, None, :] - cum), vc))
        return state, o

    init = jnp.zeros((b, h, dk, dv), jnp.float32)
    _, out = lax.scan(step, init, (to_chunks(q), to_chunks(k), to_chunks(log_f), to_chunks(v)))
    return out.transpose(1, 0, 3, 2, 4).reshape(b, s, h, dv)


def mixer_sublayer(x, ln1_g, w_in, w_branch_sb, w_branch_hg, hg_norm_g, lb, w_out):
    f32 = jnp.float32
    bsz, s, _ = x.shape
    h = rmsnorm(x, ln1_g)
    proj = h @ w_in
    widths = [SB_WIDTH] * 3 + [HG_WIDTH] * 2 + [HG_VWIDTH] * 2 + [D_MODEL]
    splits = np.cumsum(widths).tolist()
    q_sb, k_sb, v_sb, q_hg, f_hg, i_hg, g_hg, gate_sb, gate_hg = jnp.split(proj, splits, axis=-1)

    def sb_heads(t):
        return t.reshape(bsz, s, SB_HEADS, SB_HEAD_DIM).transpose(0, 2, 1, 3)
    y_sb = stick_breaking_attention(sb_heads(q_sb), sb_heads(k_sb), sb_heads(v_sb))
    y_sb = y_sb.transpose(0, 2, 1, 3).reshape(bsz, s, SB_WIDTH)

    f = lb + (1.0 - lb) * jax.nn.sigmoid(f_hg.astype(f32))
    def hg_heads(t):
        return t.reshape(bsz, s, HG_HEADS, -1)
    o = hgrn2_chunk_scan(hg_heads(jax.nn.silu(q_hg.astype(f32))), hg_heads(1.0 - f),
                         hg_heads(jnp.log(f)), hg_heads(i_hg.astype(f32)))
    o = o * lax.rsqrt(jnp.mean(o * o, axis=-1, keepdims=True) + EPS)
    o = o.reshape(bsz, s, HG_VWIDTH) * hg_norm_g.astype(f32) * jax.nn.silu(g_hg.astype(f32))
    y_hg = o.astype(x.dtype)

    merged = (jax.nn.sigmoid(gate_sb) * (y_sb @ w_branch_sb)
              + jax.nn.sigmoid(gate_hg) * (y_hg @ w_branch_hg))
    return x + merged @ w_out


def hier_moe(h, w_rg, b_rg, w_re, b_re, w_gate, w_up, w_down):
    f32 = jnp.float32
    bsz, s, d = h.shape
    t = h.reshape(-1, d)
    n = t.shape[0]
    g_logits = (t @ w_rg).astype(f32) + b_rg.astype(f32)
    p_group = jax.nn.softmax(g_logits, axis=-1)
    g_idx = jnp.argmax(g_logits, axis=-1)
    w_grp = jnp.take_along_axis(p_group, g_idx[:, None], axis=1)
    e_logits = ((t @ w_re).astype(f32) + b_re.astype(f32)).reshape(n, N_GROUPS, EXPERTS_PER_GROUP)
    in_group = jnp.take_along_axis(e_logits, g_idx[:, None, None], axis=1)[:, 0]
    top_v, top_i = lax.top_k(in_group, TOP_K_INNER)
    w_sel = jax.nn.softmax(top_v, axis=-1) * w_grp
    expert_ids = g_idx[:, None] * EXPERTS_PER_GROUP + top_i
    combine = jnp.sum(jax.nn.one_hot(expert_ids, N_EXPERTS, dtype=f32) * w_sel[..., None], axis=1)
    y = jnp.zeros((n, d), f32)
    for e in range(N_EXPERTS):
        a = jax.nn.silu(t @ w_gate[e]) * (t @ w_up[e])
        y = y + combine[:, e:e + 1] * (a @ w_down[e]).astype(f32)
    return y.astype(h.dtype).reshape(bsz, s, d)


def setup_inputs(seed: int = 0) -> dict:
    key = jax.random.key(seed)
    ks = jax.random.split(key, 17)
    f32 = jnp.float32

    def nrm(k, shape, scale):
        return jax.random.normal(k, shape, f32) * scale

    return {
        'x': nrm(ks[0], (BATCH, SEQ, D_MODEL), 1.0),
        'ln1_g': 1.0 + nrm(ks[1], (DEPTH, D_MODEL), 0.02),
        'w_in': nrm(ks[2], (DEPTH, D_MODEL, IN_COLS), D_MODEL ** -0.5),
        'w_branch_sb': nrm(ks[3], (DEPTH, SB_WIDTH, D_MODEL), SB_WIDTH ** -0.5),
        'w_branch_hg': nrm(ks[4], (DEPTH, HG_VWIDTH, D_MODEL), HG_VWIDTH ** -0.5),
        'hg_norm_g': 1.0 + nrm(ks[5], (DEPTH, HG_VWIDTH), 0.02),
        'hg_lb_logits': nrm(ks[6], (DEPTH + 1, HG_WIDTH), 0.1),
        'w_out': nrm(ks[7], (DEPTH, D_MODEL, D_MODEL), D_MODEL ** -0.5),
        'ln2_g': 1.0 + nrm(ks[8], (DEPTH, D_MODEL), 0.02),
        'w_router_group': nrm(ks[9], (DEPTH, D_MODEL, N_GROUPS), D_MODEL ** -0.5),
        'b_router_group': nrm(ks[10], (DEPTH, N_GROUPS), 0.01),
        'w_router_expert': nrm(ks[11], (DEPTH, D_MODEL, N_EXPERTS), D_MODEL ** -0.5),
        'b_router_expert': nrm(ks[12], (DEPTH, N_EXPERTS), 0.01),
        'w_exp_gate': nrm(ks[13], (DEPTH, N_EXPERTS, D_MODEL, D_EXPERT), D_MODEL ** -0.5),
        'w_exp_up': nrm(ks[14], (DEPTH, N_EXPERTS, D_MODEL, D_EXPERT), D_MODEL ** -0.5),
        'w_exp_down': nrm(ks[15], (DEPTH, N_EXPERTS, D_EXPERT, D_MODEL), D_EXPERT ** -0.5),
        'final_g': 1.0 + nrm(ks[16], (D_MODEL,), 0.02),
    }


def reference(x, ln1_g, w_in, w_branch_sb, w_branch_hg, hg_norm_g, hg_lb_logits, w_out, ln2_g,
              w_router_group, b_router_group, w_router_expert, b_router_expert,
              w_exp_gate, w_exp_up, w_exp_down, final_g):
    lb_all = jnp.cumsum(jax.nn.softmax(hg_lb_logits.astype(jnp.float32), axis=0), axis=0)
    for l in range(DEPTH):
        x = mixer_sublayer(x, ln1_g[l], w_in[l], w_branch_sb[l], w_branch_hg[l], hg_norm_g[l],
                           lb_all[l], w_out[l])
        x = x + hier_moe(rmsnorm(x, ln2_g[l]), w_router_group[l], b_router_group[l],
                         w_router_expert[l], b_router_expert[l],
                         w_exp_gate[l], w_exp_up[l], w_exp_down[l])
    return rmsnorm(x, final_g)
```

```python
import contextlib
import numpy as np
import concourse.bass as bass
import concourse.mybir as mybir
from concourse.bass_utils import run_bass_kernel_spmd

F32 = mybir.dt.float32
BF16 = mybir.dt.bfloat16
U8 = mybir.dt.uint8
AF = mybir.ActivationFunctionType
ALU = mybir.AluOpType
AX = mybir.AxisListType
PE, ACT, DVE, POOL, SP = "tensor", "scalar", "vector", "gpsimd", "sync"
ENGS = (PE, ACT, DVE, POOL, SP)
SEM_LIMIT = 30000
EPS = 1e-6
ARENA = 207 * 1024
DSZ = {F32: 4, BF16: 2, U8: 1}

D = 1024
NE = 16
DEX = 512


class Buf:
    __slots__ = ("name", "w_eng", "w_dma", "r_eng", "r_dma")

    def __init__(self, name=""):
        self.name = name
        self.w_eng = {}
        self.w_dma = []
        self.r_eng = {}
        self.r_dma = []


class Tile:
    __slots__ = ("ap", "b")

    def __init__(self, ap, b):
        self.ap = ap
        self.b = b


class Op:
    __slots__ = ("eng", "fn", "deps", "signal", "ticket", "is_dma", "prev")

    def __init__(self, eng, fn, is_dma):
        self.eng = eng
        self.fn = fn
        self.deps = []
        self.signal = is_dma
        self.ticket = None
        self.prev = None
        self.is_dma = is_dma


def _b(x):
    return x.b if isinstance(x, Tile) else x


class Prog:
    def __init__(self, nc):
        self.nc = nc
        self.ops = {e: [] for e in ENGS}
        self.stack = contextlib.ExitStack()
        self.arena = self.stack.enter_context(nc.sbuf_tensor("arena", [128, ARENA], U8))
        self.off = 0
        self.peak = 0
        self.last = {e: None for e in ENGS}
        self.dmas = []
        self.prev_dmas = []
        self.pending = {e: [] for e in ENGS}
        self.psum = []
        for i in range(8):
            t = self.stack.enter_context(nc.psum_tensor(f"ps{i}", [128, 512], F32))
            self.psum.append(Tile(t[:], Buf(f"ps{i}")))

    def alloc(self, shape, dtype, parts=128, name=""):
        n = 1
        for s in shape:
            n *= s
        nbytes = n * DSZ[dtype]
        off = (self.off + 63) // 64 * 64
        assert off + nbytes <= ARENA, f"arena overflow {name} {off + nbytes}"
        self.off = off + nbytes
        self.peak = max(self.peak, self.off)
        ap = self.arena[0:parts, off:off + nbytes].bitcast(dtype)
        if len(shape) == 2:
            ap = ap.rearrange("p (a b) -> p a b", a=shape[0])
        elif len(shape) == 3:
            ap = ap.rearrange("p (a b c) -> p a b c", a=shape[0], b=shape[1])
        return Tile(ap, Buf(name))

    def mark(self):
        return self.off

    def release(self, m):
        self.off = m
        self.barrier()

    def barrier(self):
        lasts = [o for o in self.last.values() if o is not None]
        for e in ENGS:
            self.pending[e] = list(lasts) + list(self.dmas)
        self.prev_dmas = list(self.dmas)
        self.dmas = []

    def op(self, eng, fn, reads=(), writes=(), is_dma=False):
        o = Op(eng, fn, is_dma)
        deps = []
        same_ok = not is_dma
        for x in reads:
            b = _b(x)
            deps.extend(b.w_eng.values())
            deps.extend(b.w_dma)
        for x in writes:
            b = _b(x)
            for d in list(b.w_eng.values()) + list(b.r_eng.values()):
                deps.append(d)
            deps.extend(b.w_dma)
            deps.extend(b.r_dma)
        deps.extend(self.pending[eng])
        self.pending[eng] = []
        seen = set()
        for d in deps:
            if d is o or id(d) in seen:
                continue
            seen.add(id(d))
            if d.eng == PE and eng == PE and not d.is_dma and not is_dma:
                continue
            o.deps.append(d)
            d.signal = True
        for x in reads:
            b = _b(x)
            if is_dma:
                b.r_dma.append(o)
            else:
                b.r_eng[eng] = o
        for x in writes:
            b = _b(x)
            if b.r_eng or b.r_dma:
                b.w_eng = {}
                b.w_dma = []
                b.r_eng = {}
                b.r_dma = []
            if is_dma:
                b.w_dma.append(o)
            else:
                b.w_eng[eng] = o
        self.ops[eng].append(o)
        self.last[eng] = o
        if is_dma:
            self.dmas.append(o)
        return o

    def dma(self, eng, out, in_, reads=(), writes=()):
        return self.op(eng, lambda e: e.dma_start(out=out, in_=in_), reads, writes, is_dma=True)

    def mm(self, out, lhsT, rhs, start, stop, reads=(), writes=()):
        return self.op(PE, lambda e: e.matmul(out, lhsT=lhsT, rhs=rhs, start=start, stop=stop), reads, writes)

    def tr(self, out, in_, ident, reads=(), writes=()):
        return self.op(PE, lambda e: e.transpose(out, in_, ident), reads, writes)

    def act(self, out, in_, func, reads=(), writes=(), **kw):
        return self.op(ACT, lambda e: e.activation(out=out, in_=in_, func=func, **kw), reads, writes)

    def copy(self, eng, out, in_, reads=(), writes=()):
        if eng == ACT:
            return self.op(ACT, lambda e: e.activation(out=out, in_=in_, func=AF.Copy), reads, writes)
        return self.op(eng, lambda e: e.tensor_copy(out=out, in_=in_), reads, writes)

    def tt(self, out, in0, in1, op, reads=(), writes=(), eng=DVE):
        return self.op(eng, lambda e: e.tensor_tensor(out=out, in0=in0, in1=in1, op=op), reads, writes)

    def ts(self, out, in0, s1, s2, op0, op1=None, reads=(), writes=(), eng=DVE):
        if op1 is None:
            return self.op(eng, lambda e: e.tensor_scalar(out=out, in0=in0, scalar1=s1, scalar2=None, op0=op0), reads, writes)
        return self.op(eng, lambda e: e.tensor_scalar(out=out, in0=in0, scalar1=s1, scalar2=s2, op0=op0, op1=op1), reads, writes)

    def stt(self, out, in0, scalar, in1, op0, op1, reads=(), writes=(), eng=DVE):
        return self.op(eng, lambda e: e.scalar_tensor_tensor(out=out, in0=in0, scalar=scalar, in1=in1, op0=op0, op1=op1), reads, writes)

    def recip(self, out, in_, reads=(), writes=()):
        return self.op(DVE, lambda e: e.reciprocal(out=out, in_=in_), reads, writes)

    def memset(self, out, val, writes=(), eng=DVE):
        return self.op(eng, lambda e: e.memset(out, val), (), writes)

    def reduce(self, out, in_, op, reads=(), writes=()):
        return self.op(DVE, lambda e: e.tensor_reduce(out=out, in_=in_, axis=AX.X, op=op), reads, writes)

    def emit(self, final_waits=()):
        nc = self.nc
        st = self.stack
        for o in final_waits:
            o.signal = True
        nsem = [0]

        def newsem(nm):
            nsem[0] += 1
            return st.enter_context(nc.semaphore(f"{nm}{nsem[0]}"))

        for e, lst in self.ops.items():
            cur, cnt = None, 0
            pool, pcnt, k = [], [], 0
            for o in lst:
                if not o.signal:
                    continue
                if o.is_dma:
                    if len(pool) < 16:
                        pool.append(newsem("d" + e[:2]))
                        pcnt.append(0)
                    j = k % len(pool)
                    k += 1
                    if pcnt[j] + 16 > SEM_LIMIT:
                        pool[j] = newsem("d" + e[:2])
                        pcnt[j] = 0
                    if pcnt[j] > 0:
                        o.prev = (pool[j], pcnt[j])
                    pcnt[j] += 16
                    o.ticket = (pool[j], pcnt[j])
                else:
                    if cur is None or cnt >= SEM_LIMIT:
                        cur = newsem("c" + e[:2])
                        cnt = 0
                    cnt += 1
                    o.ticket = (cur, cnt)
        self.nsem = nsem[0]
        finals = list(final_waits)

        with nc.Block() as block:
            def make(ename):
                lst = self.ops[ename]

                def body(eng):
                    waited = {}
                    for o in lst:
                        need = {}
                        tl = [d.ticket for d in o.deps]
                        if o.prev is not None:
                            tl.append(o.prev)
                        for sem, val in tl:
                            k = id(sem)
                            if k not in need or need[k][1] < val:
                                need[k] = (sem, val)
                        for k, (sem, val) in need.items():
                            if waited.get(k, 0) >= val:
                                continue
                            waited[k] = val
                            eng.wait_ge(sem, val)
                        ins = o.fn(eng)
                        if o.signal:
                            ins.then_inc(o.ticket[0], 16 if o.is_dma else 1)
                    if ename == SP:
                        for d in finals:
                            eng.wait_ge(d.ticket[0], d.ticket[1])
                return body

            block.sync(make(SP))
            block.tensor(make(PE))
            block.scalar(make(ACT))
            block.vector(make(DVE))
            block.gpsimd(make(POOL))
        st.close()


class Ring:
    def __init__(self, tiles):
        self.tiles = tiles
        self.i = 0

    def next(self):
        t = self.tiles[self.i % len(self.tiles)]
        self.i += 1
        return t


def run_tasks(tasks, pools):
    free = {k: list(v) for k, v in pools.items()}
    done = [False] * len(tasks)
    active = []
    nxt = 0
    while nxt < len(tasks) or active:
        while nxt < len(tasks):
            t = tasks[nxt]
            if not all(done[d] for d in t["deps"]) or not free[t["kind"]]:
                break
            sl = free[t["kind"]].pop(0)
            active.append((nxt, t["gen"](sl), sl))
            nxt += 1
        assert active, "task deadlock"
        keep = []
        for idx, g, sl in active:
            try:
                next(g)
                keep.append((idx, g, sl))
            except StopIteration:
                done[idx] = True
                if tasks[idx].get("fin"):
                    tasks[idx]["fin"](sl)
                free[tasks[idx]["kind"]].append(sl)
        active = keep


C_ID = 0
C_NTRI = 128
C_KPOS = 256
C_TRI64 = 320
C_SUP64 = 384
C_SUP128 = 448
NCONST = 576


def make_consts():
    c = np.zeros((128, NCONST), np.float32)
    j = np.arange(128)
    c[:, C_ID:C_ID + 128] = np.eye(128)
    c[:, C_NTRI:C_NTRI + 128] = -(j[:, None] >= j[None, :]).astype(np.float32)
    c[:, C_KPOS:C_KPOS + 64] = np.arange(64)[None, :] * 128 + j[:, None]
    s = np.arange(64)
    c[:64, C_TRI64:C_TRI64 + 64] = (s[:, None] <= s[None, :])
    c[:64, C_SUP64:C_SUP64 + 64] = (s[:, None] > s[None, :])
    c[:, C_SUP128:C_SUP128 + 128] = (j[:, None] > j[None, :])
    return c


def build_program(stop_after=None, debug=False, nT=16, nslots=4, b1_level=2, sim_softplus=False):
    nc = bass.Bass("TRN2", target_bir_lowering=False)
    P = Prog(nc)

    def din(n, s):
        return nc.dram_tensor(n, list(s), F32, kind="ExternalInput").ap()

    x_all = din("x_all", [8192, D])
    x_own = din("x_own", [2048, D])
    qpos_d = din("qpos", [128, 2048])
    sel_d = din("sel", [64, 64])
    consts_d = din("consts", [128, NCONST])
    w_in = din("w_in", [D, 5632])
    w_bsb = din("w_bsb", [512, D])
    w_bhg = din("w_bhg", [512, D])
    w_out = din("w_out", [D, D])
    w_rt = din("w_rt", [D, 20])
    b_rt = din("b_rt", [20])
    w_eg = din("w_eg", [NE, D, DEX])
    w_eu = din("w_eu", [NE, D, DEX])
    w_ed = din("w_ed", [NE, DEX, D])
    ln1_d = din("ln1_g", [D])
    ln2_d = din("ln2_g", [D])
    fin_d = din("final_g", [D])
    hgn_d = din("hg_norm_g", [512])
    lbl_d = din("hg_lb_logits", [2, 512])
    out_d = nc.dram_tensor("out", [2048, D], F32, kind="ExternalOutput").ap()
    kd_kind = "ExternalOutput" if debug else "Internal"
    KT_d = nc.dram_tensor("KT_d", [4, 128, 8192], BF16, kind=kd_kind).ap()
    V_d = nc.dram_tensor("V_d", [4, 64, 128, 128], BF16, kind=kd_kind).ap()
    x1_d = nc.dram_tensor("x1_d", [2048, D], F32, kind=kd_kind).ap()
    dbg = {}
    if debug:
        dbg["ssel"] = nc.dram_tensor("dbg_ssel", [64, 4 * 512], F32, kind="ExternalOutput").ap()
        dbg["qt"] = nc.dram_tensor("dbg_qt", [128, 4 * 2048], BF16, kind="ExternalOutput").ap()
        dbg["yhg"] = nc.dram_tensor("dbg_yhg", [128, 4 * 2048], BF16, kind="ExternalOutput").ap()
        dbg["ysb"] = nc.dram_tensor("dbg_ysb", [128, 4 * 2048], BF16, kind="ExternalOutput").ap()
    bKT, bV, bx1 = Buf("KT_d"), Buf("V_d"), Buf("x1_d")
    finals = []

    def wslice(c0, n):
        return w_in[:, c0:c0 + n].rearrange("(c p) n -> p c n", p=128)

    cst = P.alloc([NCONST], F32, name="cst")
    P.dma(SP, cst.ap, consts_d[:, :], writes=[cst])
    cbf = P.alloc([384], BF16, name="cbf")
    P.dma(POOL, cbf.ap[:, 0:256], consts_d[:, 0:256], writes=[cbf])
    P.memset(cbf.ap[:, 256:384], -1.0, writes=[cbf])
    negones128 = cbf.ap[:, 256:384]
    ident_bf = cbf.ap[:, 0:128]
    negtri_bf = cbf.ap[:, 128:256]
    ident_f = cst.ap[:, C_ID:C_ID + 128]
    small = P.alloc([16], F32, name="small")
    P.memset(small.ap[:, 0:1], EPS, writes=[small])
    P.memset(small.ap[:, 1:2], 1.0, writes=[small])
    eps_col = small.ap[:, 0:1]
    ones_f = small.ap[:, 1:2]
    smallb = P.alloc([132], BF16, name="smallb")
    P.memset(smallb.ap[:, 0:1], 1.0, writes=[smallb])
    P.memset(smallb.ap[:, 4:132], -1.0, writes=[smallb])
    ones_bf_col = smallb.ap[:, 0:1]
    negones_row = smallb.ap[0:1, 4:132]
    junk = P.alloc([D], BF16, name="junk")
    stat = Ring([P.alloc([8], F32, name=f"stat{i}") for i in range(4)])
    glob_mark = P.mark()
    ln1_b = P.alloc([D], F32, name="ln1_b")
    P.dma(SP, ln1_b.ap, ln1_d.partition_broadcast(128), writes=[ln1_b])
    hgn_b = P.alloc([512], F32, name="hgn_b")
    P.dma(SP, hgn_b.ap, hgn_d.partition_broadcast(128), writes=[hgn_b])
    lb_b = P.alloc([512], F32, name="lb_b")
    oml_b = P.alloc([512], F32, name="oml_b")
    Ssel = P.alloc([4, 512], F32, parts=64, name="Ssel")
    P.memset(Ssel.ap, 0.0, writes=[Ssel])
    sel_t = P.alloc([64], F32, parts=64, name="sel")
    P.dma(SP, sel_t.ap, sel_d[:, :], writes=[sel_t])
    base_mark = P.mark()
    lbt = P.alloc([2, 512], F32, name="lbt")
    P.dma(SP, lbt.ap[:, 0, :], lbl_d[0, :].partition_broadcast(128), writes=[lbt])
    P.dma(SP, lbt.ap[:, 1, :], lbl_d[1, :].partition_broadcast(128), writes=[lbt])
    tmpl = P.alloc([512], F32, name="tmpl")
    P.tt(tmpl.ap, lbt.ap[:, 1, :], lbt.ap[:, 0, :], ALU.subtract, reads=[lbt], writes=[tmpl])
    P.act(tmpl.ap, tmpl.ap, AF.Exp, reads=[tmpl], writes=[tmpl])
    P.ts(tmpl.ap, tmpl.ap, 1.0, None, ALU.add, reads=[tmpl], writes=[tmpl])
    P.recip(lb_b.ap, tmpl.ap, reads=[tmpl], writes=[lb_b])
    P.ts(oml_b.ap, lb_b.ap, -1.0, 1.0, ALU.mult, ALU.add, reads=[lb_b], writes=[oml_b])

    psr = {"i": 0}

    def ps_next(lo=2, hi=8):
        k = lo + psr["i"] % (hi - lo)
        psr["i"] += 1
        return P.psum[k]

    tpr = {"i": 0}

    def tp_next():
        k = tpr["i"] % 2
        tpr["i"] += 1
        return P.psum[k]

    def rms_stats(src_ap, src_t, n, parts=128):
        s = stat.next()
        sa = s.ap[0:parts]
        P.memset(sa[:, 0:1], 0.0, writes=[s])
        P.act(junk.ap[0:parts, 0:n], src_ap, AF.Square, reads=[src_t, s], writes=[junk, s], accum_out=sa[:, 0:1])
        P.act(sa[:, 1:2], sa[:, 0:1], AF.Ln, reads=[s], writes=[s], scale=1.0 / n, bias=eps_col[0:parts])
        P.act(sa[:, 2:3], sa[:, 1:2], AF.Exp, reads=[s], writes=[s], scale=-0.5)
        return s

    def norm_transpose(xt, g_b, hb, hT_dst, hT_t):
        s = rms_stats(xt.ap, xt, D)
        P.stt(hb.ap, xt.ap, s.ap[:, 2:3], g_b.ap, ALU.mult, ALU.mult, reads=[xt, s, g_b], writes=[hb])
        tp = tp_next()
        tpv = tp.ap.bitcast(BF16).rearrange("p (a b) -> p a b", b=128)
        for c in range(8):
            P.tr(tpv[:, c, :], hb.ap[:, c * 128:(c + 1) * 128], ident_bf, reads=[hb, cbf], writes=[tp])
        P.copy(ACT, hT_dst, tpv, reads=[tp], writes=[hT_t])

    def proj_tok(ps, hT_ap, hT_t, tok0, ntok, W, reads_extra=()):
        for c in range(8):
            P.mm(ps.ap[0:ntok, :], hT_ap[:, c, tok0:tok0 + ntok], W.ap[:, c, :], c == 0, c == 7,
                 reads=[hT_t, W], writes=[ps])

    def sigmoid_from(ps_ap, ps_t, parts, out_t, tmp_t):
        P.act(tmp_t.ap[0:parts], ps_ap, AF.Exp, reads=[ps_t], writes=[tmp_t], scale=-1.0)
        P.act(tmp_t.ap[0:parts], tmp_t.ap[0:parts], AF.Ln, reads=[tmp_t], writes=[tmp_t], bias=1.0, scale=1.0)
        P.act(out_t.ap[0:parts], tmp_t.ap[0:parts], AF.Exp, reads=[tmp_t], writes=[out_t], scale=-1.0)

    mA = base_mark
    Wk = P.alloc([8, 512], BF16, name="Wk")
    Wv = P.alloc([8, 512], BF16, name="Wv")
    Wf = P.alloc([8, 512], BF16, name="Wf")
    Wi = P.alloc([8, 512], BF16, name="Wi")
    P.dma(POOL, Wk.ap, wslice(512, 512), writes=[Wk])
    P.dma(POOL, Wv.ap, wslice(1024, 512), writes=[Wv])
    P.dma(POOL, Wf.ap, wslice(2048, 512), writes=[Wf])
    P.dma(POOL, Wi.ap, wslice(2560, 512), writes=[Wi])
    hT_tiles = [P.alloc([8, 512], BF16, name=f"hT{i}") for i in range(2)]
    S = P.alloc([8, 64], F32, parts=64, name="S")
    P.memset(S.ap, 0.0, writes=[S])
    sup128 = cst.ap[:, C_SUP128:C_SUP128 + 128]
    Fslots = [dict(x=[P.alloc([D], F32, name=f"xa{k}{i}") for i in range(4)],
                   hb=[P.alloc([D], BF16, name=f"hba{k}{i}") for i in range(2)],
                   st=[P.alloc([8], F32, name=f"sta{k}{i}") for i in range(2)],
                   KTt=P.alloc([4, 512], BF16, name=f"KTt{k}"), Vt=P.alloc([4, 512], BF16, name=f"Vt{k}"),
                   tp=P.psum[k], ps=P.psum[2 + k]) for k in range(2)]
    Hslots = [dict(f32=[P.alloc([512], F32, name=f"fa{k}{i}") for i in range(4)],
                   bf=[P.alloc([512], BF16, name=f"ba{k}{i}") for i in range(2)],
                   et=P.alloc([8], F32, parts=64, name=f"et{k}"),
                   usb=P.alloc([512], F32, parts=64, name=f"us{k}"), ps=P.psum[4 + k]) for k in range(4)]

    def ssel_acc(T):
        for i in range(4):
            col = i * 16 + T
            P.stt(Ssel.ap[:, i, :], S.ap.rearrange("p h d -> p (h d)"), sel_t.ap[:, col:col + 1], Ssel.ap[:, i, :],
                  ALU.mult, ALU.add, reads=[S, sel_t, Ssel], writes=[Ssel])

    def frontA(T):
        def gen(sl):
            hT = hT_tiles[T % 2]
            xts = sl["x"]
            tp, ps = sl["tp"], sl["ps"]
            tpv = tp.ap.bitcast(BF16).rearrange("p (a b) -> p a b", b=128)
            for blk in range(4):
                r0 = T * 512 + blk * 128
                P.dma(SP, xts[blk].ap, x_all[r0:r0 + 128, :], writes=[xts[blk]])
            yield
            for blk in range(4):
                xt, hb, s_ = xts[blk], sl["hb"][blk % 2], sl["st"][blk % 2]
                P.memset(s_.ap[:, 0:1], 0.0, writes=[s_])
                P.act(junk.ap, xt.ap, AF.Square, reads=[xt, s_], writes=[junk, s_], accum_out=s_.ap[:, 0:1])
                P.act(s_.ap[:, 1:2], s_.ap[:, 0:1], AF.Ln, reads=[s_], writes=[s_], scale=1.0 / D, bias=eps_col)
                P.act(s_.ap[:, 2:3], s_.ap[:, 1:2], AF.Exp, reads=[s_], writes=[s_], scale=-0.5)
                yield
                P.stt(hb.ap, xt.ap, s_.ap[:, 2:3], ln1_b.ap, ALU.mult, ALU.mult, reads=[xt, s_, ln1_b], writes=[hb])
                yield
                for c in range(8):
                    P.tr(tpv[:, c, :], hb.ap[:, c * 128:(c + 1) * 128], ident_bf, reads=[hb, cbf], writes=[tp])
                yield
                P.copy(ACT if blk % 2 else DVE, hT.ap[:, :, blk * 128:(blk + 1) * 128], tpv, reads=[tp], writes=[hT])
                yield
            KTt = sl["KTt"]
            for p in range(4):
                for c in range(8):
                    P.mm(ps.ap, Wk.ap[:, c, p * 128:(p + 1) * 128], hT.ap[:, c, :], c == 0, c == 7,
                         reads=[Wk, hT], writes=[ps])
                yield
                P.copy(ACT if p % 2 else DVE, KTt.ap[:, p, :], ps.ap, reads=[ps], writes=[KTt])
                yield
            P.dma(POOL, KT_d[:, :, T * 512:(T + 1) * 512].rearrange("q p t -> p q t"), KTt.ap, reads=[KTt], writes=[bKT])
            Vt = sl["Vt"]
            for blk in range(4):
                proj_tok(ps, hT.ap, hT, blk * 128, 128, Wv)
                yield
                P.copy(DVE if blk % 2 else ACT, Vt.ap[:, blk, :], ps.ap, reads=[ps], writes=[Vt])
                yield
            for blk in range(4):
                P.dma(POOL, V_d[:, T * 4 + blk, :, :].rearrange("p k c -> k p c"),
                      Vt.ap[:, blk, :].rearrange("k (p c) -> k p c", p=4), reads=[Vt], writes=[bV])
        return gen

    def hgA(T, blk):
        def gen(sl):
            hT = hT_tiles[T % 2]
            sg, tmp, f, g = sl["f32"]
            ibf, kd = sl["bf"]
            et, usb, ps = sl["et"], sl["usb"], sl["ps"]
            proj_tok(ps, hT.ap, hT, blk * 128, 128, Wf)
            yield
            sigmoid_from(ps.ap, ps, 128, sg, tmp)
            yield
            proj_tok(ps, hT.ap, hT, blk * 128, 128, Wi)
            yield
            P.copy(DVE, ibf.ap, ps.ap, reads=[ps], writes=[ibf])
            P.tt(f.ap, sg.ap, oml_b.ap, ALU.mult, reads=[sg, oml_b], writes=[f])
            P.tt(f.ap, f.ap, lb_b.ap, ALU.add, reads=[f, lb_b], writes=[f])
            yield
            P.act(g.ap, f.ap, AF.Ln, reads=[f], writes=[g])
            kk = sg
            P.ts(kk.ap, f.ap, -1.0, 1.0, ALU.mult, ALU.add, reads=[f], writes=[kk])
            yield
            P.mm(ps.ap, sup128, g.ap, True, True, reads=[cst, g], writes=[ps])
            yield
            ed = tmp
            P.act(ed.ap, ps.ap, AF.Exp, reads=[ps], writes=[ed])
            yield
            for h in range(8):
                P.mm(ps.ap[0:64, h:h + 1], g.ap[:, h * 64:(h + 1) * 64], ones_f, True, True,
                     reads=[g, small], writes=[ps])
            yield
            P.act(et.ap, ps.ap[0:64, 0:8], AF.Exp, reads=[ps], writes=[et])
            P.tt(kd.ap, kk.ap, ed.ap, ALU.mult, reads=[kk, ed], writes=[kd])
            yield
            for h in range(8):
                P.mm(ps.ap[0:64, h * 64:(h + 1) * 64], kd.ap[:, h * 64:(h + 1) * 64], ibf.ap[:, h * 64:(h + 1) * 64],
                     True, True, reads=[kd, ibf], writes=[ps])
            yield
            P.copy(ACT, usb.ap, ps.ap[0:64, :], reads=[ps], writes=[usb])

        def fin(sl):
            et, usb = sl["et"], sl["usb"]
            if blk == 0:
                ssel_acc(T)
            P.tt(S.ap, S.ap, et.ap.unsqueeze(2).to_broadcast([64, 8, 64]), ALU.mult, reads=[S, et], writes=[S])
            P.tt(S.ap, S.ap, usb.ap.rearrange("p (h d) -> p h d", h=8), ALU.add, reads=[S, usb], writes=[S])
        return gen, fin

    tasksA = []
    fidx = {}
    hidx = {}

    def addF(T):
        fidx[T] = len(tasksA)
        tasksA.append(dict(kind="F", gen=frontA(T), deps=list(hidx.get(T - 2, [])), fin=None))

    addF(0)
    for T in range(nT):
        if T + 1 < nT:
            addF(T + 1)
        if T < nT - 1:
            hidx[T] = []
            for blk in range(4):
                g_, f_ = hgA(T, blk)
                hidx[T].append(len(tasksA))
                tasksA.append(dict(kind="H", gen=g_, deps=[fidx[T]], fin=f_))
    run_tasks(tasksA, {"F": Fslots, "H": Hslots})
    ssel_acc(nT - 1)
    if debug:
        finals.append(P.dma(SP, dbg["ssel"][:, :], Ssel.ap.rearrange("p a b -> p (a b)"), reads=[Ssel]))
    P.release(mA)
    if stop_after == "A":
        P.emit(final_waits=finals + list(P.prev_dmas) + list(P.dmas))
        return nc

    QT_all = P.alloc([4, 2048], BF16, name="QT_all")
    yhgT_all = P.alloc([4, 2048], BF16, name="yhgT_all")
    mB1 = P.mark()
    Wq = P.alloc([8, 512], BF16, name="Wq")
    Wqh = P.alloc([8, 512], BF16, name="Wqh")
    Wf = P.alloc([8, 512], BF16, name="Wf")
    Wi = P.alloc([8, 512], BF16, name="Wi")
    Wg = P.alloc([8, 512], BF16, name="Wg")
    P.dma(POOL, Wq.ap, wslice(0, 512), writes=[Wq])
    P.dma(POOL, Wqh.ap, wslice(1536, 512), writes=[Wqh])
    P.dma(POOL, Wf.ap, wslice(2048, 512), writes=[Wf])
    P.dma(POOL, Wi.ap, wslice(2560, 512), writes=[Wi])
    P.dma(POOL, Wg.ap, wslice(3072, 512), writes=[Wg])
    hT_tiles = [P.alloc([8, 512], BF16, name=f"hTb{i}") for i in range(2)]
    FBslots = [dict(x=[P.alloc([D], F32, name=f"xb{i}") for i in range(4)],
                    hb=[P.alloc([D], BF16, name=f"hbb{i}") for i in range(1)] * 2,
                    st=[P.alloc([8], F32, name=f"stb{i}") for i in range(2)],
                    tp=P.psum[0], ps=P.psum[1])]
    CHslots = [dict(f32=[P.alloc([512], F32, parts=64, name=f"fb{k}{i}") for i in range(7)],
                    qd=P.alloc([512], BF16, parts=64, name=f"qd{k}"), kdn=P.alloc([512], BF16, parts=64, name=f"kdn{k}"),
                    kT=P.alloc([8, 64], BF16, parts=64, name=f"kT{k}"),
                    qdT=P.alloc([8, 64], BF16, parts=64, name=f"qdT{k}"), scT=P.alloc([8, 64], BF16, parts=64, name=f"scT{k}"),
                    ibf=P.alloc([512], BF16, parts=64, name=f"ibf{k}"), kd=P.alloc([512], BF16, parts=64, name=f"kd{k}"),
                    sgt=P.alloc([512], BF16, parts=64, name=f"sgt{k}"), ybf=P.alloc([512], BF16, parts=64, name=f"ybf{k}"),
                    et=P.alloc([8], F32, parts=64, name=f"etc{k}"), s8=P.alloc([24], F32, parts=64, name=f"s8{k}"),
                    ps=P.psum[2 + 2 * k], tp=P.psum[3 + 2 * k])
               for k in range(3)]
    S_t = [P.alloc([8, 64], F32, parts=64, name=f"S1{i}") for i in range(2)]
    Sbf_t = [P.alloc([8, 64], BF16, parts=64, name=f"Sbf{i}") for i in range(2)]
    tri64 = cst.ap[0:64, C_TRI64:C_TRI64 + 64]
    sup64 = cst.ap[0:64, C_SUP64:C_SUP64 + 64]
    id64 = ident_bf[0:64, 0:64]
    turn = {"n": 0}

    def frontB(i):
        def gen(sl):
            hT = hT_tiles[i % 2]
            xts = sl["x"]
            tp, ps = sl["tp"], sl["ps"]
            tpv = tp.ap.bitcast(BF16).rearrange("p (a b) -> p a b", b=128)
            for blk in range(4):
                r0 = i * 512 + blk * 128
                P.dma(SP, xts[blk].ap, x_own[r0:r0 + 128, :], writes=[xts[blk]])
            yield
            for blk in range(4):
                xt, hb, s_ = xts[blk], sl["hb"][blk % 2], sl["st"][blk % 2]
                P.memset(s_.ap[:, 0:1], 0.0, writes=[s_])
                P.act(junk.ap, xt.ap, AF.Square, reads=[xt, s_], writes=[junk, s_], accum_out=s_.ap[:, 0:1])
                P.act(s_.ap[:, 1:2], s_.ap[:, 0:1], AF.Ln, reads=[s_], writes=[s_], scale=1.0 / D, bias=eps_col)
                P.act(s_.ap[:, 2:3], s_.ap[:, 1:2], AF.Exp, reads=[s_], writes=[s_], scale=-0.5)
                yield
                P.stt(hb.ap, xt.ap, s_.ap[:, 2:3], ln1_b.ap, ALU.mult, ALU.mult, reads=[xt, s_, ln1_b], writes=[hb])
                yield
                for c in range(8):
                    P.tr(tpv[:, c, :], hb.ap[:, c * 128:(c + 1) * 128], ident_bf, reads=[hb, cbf], writes=[tp])
                yield
                P.copy(ACT if blk % 2 else DVE, hT.ap[:, :, blk * 128:(blk + 1) * 128], tpv, reads=[tp], writes=[hT])
                yield
            for p in range(4):
                for c in range(8):
                    P.mm(ps.ap, Wq.ap[:, c, p * 128:(p + 1) * 128], hT.ap[:, c, :], c == 0, c == 7,
                         reads=[Wq, hT], writes=[ps])
                yield
                P.act(QT_all.ap[:, p, i * 512:(i + 1) * 512], ps.ap, AF.Copy, reads=[ps], writes=[QT_all], scale=0.125)
                yield
        return gen

    def chunkB(i, ch):
        myturn = i * 8 + ch

        def gen(sl):
            hT = hT_tiles[i % 2]
            S, Sbf_r, Sbf_w = S_t[i % 2], Sbf_t[ch % 2], Sbf_t[(ch + 1) % 2]
            t0 = ch * 64
            t_sg, t_tmp, t_f, t_g, t_rq, t_qs, t_er = sl["f32"]
            t_us = t_er
            qd, kdn, kT, qdT, scT, ibf, kd, sgt, ybf, et, s8 = (
                sl[k] for k in ("qd", "kdn", "kT", "qdT", "scT", "ibf", "kd", "sgt", "ybf", "et", "s8"))
            ps, tp = sl["ps"], sl["tp"]
            P64 = ps.ap[0:64, :]
            proj_tok(ps, hT.ap, hT, t0, 64, Wf)
            yield
            sigmoid_from(P64, ps, 64, t_sg, t_tmp)
            yield
            P.tt(t_f.ap, t_sg.ap, oml_b.ap[0:64], ALU.mult, reads=[t_sg, oml_b], writes=[t_f])
            P.tt(t_f.ap, t_f.ap, lb_b.ap[0:64], ALU.add, reads=[t_f, lb_b], writes=[t_f])
            kk = t_sg
            P.ts(kk.ap, t_f.ap, -1.0, 1.0, ALU.mult, ALU.add, reads=[t_f], writes=[kk])
            proj_tok(ps, hT.ap, hT, t0, 64, Wqh)
            yield
            P.act(t_g.ap, t_f.ap, AF.Ln, reads=[t_f], writes=[t_g])
            sigmoid_from(P64, ps, 64, t_rq, t_tmp)
            yield
            P.tt(t_qs.ap, t_rq.ap, P64, ALU.mult, reads=[t_rq, ps], writes=[t_qs])
            yield
            proj_tok(ps, hT.ap, hT, t0, 64, Wg)
            yield
            sigmoid_from(P64, ps, 64, t_rq, t_tmp)
            yield
            P.tt(sgt.ap, t_rq.ap, P64, ALU.mult, reads=[t_rq, ps], writes=[sgt])
            yield
            proj_tok(ps, hT.ap, hT, t0, 64, Wi)
            yield
            P.copy(ACT, ibf.ap, P64, reads=[ps], writes=[ibf])
            yield
            ecum, encum, erev = t_rq, t_tmp, t_er
            P.mm(P64, tri64, t_g.ap, True, True, reads=[cst, t_g], writes=[ps])
            yield
            P.act(ecum.ap, P64, AF.Exp, reads=[ps], writes=[ecum])
            P.act(encum.ap, P64, AF.Exp, reads=[ps], writes=[encum], scale=-1.0)
            yield
            P.mm(P64, sup64, t_g.ap, True, True, reads=[cst, t_g], writes=[ps])
            P.tt(qd.ap, t_qs.ap, ecum.ap, ALU.mult, reads=[t_qs, ecum], writes=[qd])
            P.tt(kdn.ap, kk.ap, encum.ap, ALU.mult, reads=[kk, encum], writes=[kdn])
            yield
            P.act(erev.ap, P64, AF.Exp, reads=[ps], writes=[erev])
            yield
            for h in range(8):
                P.mm(ps.ap[0:64, h:h + 1], t_g.ap[:, h * 64:(h + 1) * 64], ones_f[0:64], True, True,
                     reads=[t_g, small], writes=[ps])
            P.tt(kd.ap, kk.ap, erev.ap, ALU.mult, reads=[kk, erev], writes=[kd])
            tqv = tp.ap.bitcast(BF16)[0:64, 0:512].rearrange("p (h t) -> p h t", h=8)
            for h in range(8):
                P.tr(tqv[:, h, :], qd.ap[:, h * 64:(h + 1) * 64], id64, reads=[qd, cbf], writes=[tp])
            yield
            P.act(et.ap, ps.ap[0:64, 0:8], AF.Exp, reads=[ps], writes=[et])
            P.copy(ACT, qdT.ap, tqv, reads=[tp], writes=[qdT])
            yield
            for h in range(8):
                P.mm(ps.ap[0:64, h * 64:(h + 1) * 64], kd.ap[:, h * 64:(h + 1) * 64], ibf.ap[:, h * 64:(h + 1) * 64],
                     True, True, reads=[kd, ibf], writes=[ps])
            for h in range(8):
                P.tr(tqv[:, h, :], kdn.ap[:, h * 64:(h + 1) * 64], id64, reads=[kdn, cbf], writes=[tp])
            yield
            P.copy(ACT, t_us.ap, P64, reads=[ps], writes=[t_us])
            P.copy(DVE, kT.ap, tqv, reads=[tp], writes=[kT])
            yield
            for h in range(8):
                P.mm(ps.ap[0:64, h * 64:(h + 1) * 64], kT.ap[:, h, :], qdT.ap[:, h, :], True, True,
                     reads=[kT, qdT], writes=[ps])
            yield
            P.tt(scT.ap, P64.rearrange("p (h t) -> p h t", h=8),
                 tri64.unsqueeze(1).to_broadcast([64, 8, 64]), ALU.mult, reads=[ps, cst], writes=[scT])
            yield
            while turn["n"] != myturn:
                yield
            if ch == 0:
                P.copy(DVE, S.ap.rearrange("p h d -> p (h d)"), Ssel.ap[:, i, :], reads=[Ssel], writes=[S])
                P.copy(DVE, Sbf_r.ap, S.ap, reads=[S], writes=[Sbf_r])
            for h in range(8):
                hs = slice(h * 64, (h + 1) * 64)
                P.mm(ps.ap[0:64, hs], scT.ap[:, h, :], ibf.ap[:, hs], True, False, reads=[scT, ibf], writes=[ps])
                P.mm(ps.ap[0:64, hs], qdT.ap[:, h, :], Sbf_r.ap[:, h, :], False, True, reads=[qdT, Sbf_r], writes=[ps])
            if ch < 7:
                P.tt(S.ap, S.ap, et.ap.unsqueeze(2).to_broadcast([64, 8, 64]), ALU.mult, reads=[S, et], writes=[S])
                P.tt(S.ap, S.ap, t_us.ap.rearrange("p (h d) -> p h d", h=8), ALU.add, reads=[S, t_us], writes=[S])
                P.copy(DVE, Sbf_w.ap, S.ap, reads=[S], writes=[Sbf_w])
            turn["n"] += 1
            yield
            osb, sq, y1 = t_f, t_g, t_qs
            P.copy(ACT, osb.ap, P64, reads=[ps], writes=[osb])
            yield
            P.tt(sq.ap, osb.ap, osb.ap, ALU.mult, reads=[osb], writes=[sq])
            P.reduce(s8.ap[:, 0:8], sq.ap.rearrange("p (h d) -> p h d", h=8), ALU.add, reads=[sq], writes=[s8])
            yield
            P.act(s8.ap[:, 8:16], s8.ap[:, 0:8], AF.Ln, reads=[s8], writes=[s8], scale=1.0 / 64, bias=eps_col[0:64])
            P.act(s8.ap[:, 16:24], s8.ap[:, 8:16], AF.Exp, reads=[s8], writes=[s8], scale=-0.5)
            yield
            P.tt(y1.ap.rearrange("p (h d) -> p h d", h=8), osb.ap.rearrange("p (h d) -> p h d", h=8),
                 s8.ap[:, 16:24].unsqueeze(2).to_broadcast([64, 8, 64]), ALU.mult, reads=[osb, s8], writes=[y1])
            P.tt(y1.ap, y1.ap, hgn_b.ap[0:64], ALU.mult, reads=[y1, hgn_b], writes=[y1])
            P.tt(ybf.ap, y1.ap, sgt.ap, ALU.mult, reads=[y1, sgt], writes=[ybf])
            yield
            tyv = tp.ap.bitcast(BF16)[:, 0:256].rearrange("p (c t) -> p c t", c=4)
            for c4 in range(4):
                P.tr(tyv[:, c4, :], ybf.ap[:, c4 * 128:(c4 + 1) * 128], id64, reads=[ybf, cbf], writes=[tp])
            yield
            tok = i * 512 + ch * 64
            P.copy(ACT, yhgT_all.ap[:, :, tok:tok + 64], tyv, reads=[tp], writes=[yhgT_all])
        return gen, None

    tasksB = []
    fb = {}
    cb = {}

    def addFB(i):
        fb[i] = len(tasksB)
        tasksB.append(dict(kind="F", gen=frontB(i), deps=list(cb.get(i - 2, [])), fin=None))

    addFB(0)
    for i in range(nslots):
        if i + 1 < nslots:
            addFB(i + 1)
        cb[i] = []
        if b1_level >= 1:
            for ch in range(8):
                g_, f_ = chunkB(i, ch)
                cb[i].append(len(tasksB))
                tasksB.append(dict(kind="CH", gen=g_, deps=[fb[i]], fin=f_))
    run_tasks(tasksB, {"F": FBslots, "CH": CHslots})
    if debug:
        finals.append(P.dma(SP, dbg["qt"][:, :], QT_all.ap.rearrange("p a b -> p (a b)"), reads=[QT_all]))
        finals.append(P.dma(SP, dbg["yhg"][:, :], yhgT_all.ap.rearrange("p a b -> p (a b)"), reads=[yhgT_all]))
    P.release(mB1)
    if stop_after == "B1":
        P.emit(final_waits=finals + list(P.prev_dmas) + list(P.dmas))
        return nc

    ysbT_all = P.alloc([4, 2048], BF16, name="ysbT_all")
    mB2 = P.mark()
    qpos_b = P.alloc([2048], F32, name="qpos_b")
    P.dma(SP, qpos_b.ap, qpos_d[:, :], writes=[qpos_b])
    KTp = [P.alloc([8192], BF16, name=f"KTp{i}") for i in range(2)]
    Vp = [P.alloc([64, 128], BF16, name=f"Vp{i}") for i in range(2)]
    kpos = cst.ap[:, C_KPOS:C_KPOS + 64]
    chain = []
    for j in range(2):
        chain.append(dict(
            u=Ring([P.alloc([512], BF16, name=f"u{j}{k}") for k in range(2)]),
            Lt=Ring([P.alloc([512], BF16, name=f"Lt{j}{k}") for k in range(2)]),
            Lm=Ring([P.alloc([512], BF16, name=f"Lm{j}{k}") for k in range(3)]),
            At=Ring([P.alloc([512], BF16, name=f"At{j}{k}") for k in range(2)]),
            A=Ring([P.alloc([512], BF16, name=f"A{j}{k}") for k in range(3)]),
            LS=[P.alloc([512], BF16, name=f"LS{j}{k}") for k in range(3)],
            Z1=[P.psum[2 * j], P.psum[6 + j]],
            Z2=P.psum[2 * j + 1],
            O=P.psum[4 + j],
        ))

    def load_pair(p):
        kt, vp = KTp[p % 2], Vp[p % 2]
        P.dma(SP, kt.ap, KT_d[p], reads=[bKT], writes=[kt])
        for q in range(4):
            P.dma(SP, vp.ap[:, q * 16:(q + 1) * 16, :], V_d[p, q * 16:(q + 1) * 16, :, :].rearrange("b k c -> k b c"),
                  reads=[bV], writes=[vp])

    load_pair(0)
    for p in range(4):
        if p + 1 < 4:
            load_pair(p + 1)
        kt, vp = KTp[p % 2], Vp[p % 2]
        for i in range(4):
            nk = 16 * (i + 1)
            nmask0 = 16 * i
            qs_ = slice(i * 512, (i + 1) * 512)
            st = [dict(), dict()]
            st3 = [dict(), dict()]
            st1 = [dict(), dict()]

            def S1(j, n):
                cj = chain[j]
                kb = nk - 1 - n
                Z1 = cj["Z1"][n % 2]
                js = slice(j * 64, (j + 1) * 64)
                P.mm(Z1.ap, kt.ap[js, kb * 128:(kb + 1) * 128], QT_all.ap[js, p, qs_], True, True,
                     reads=[kt, QT_all], writes=[Z1])
                u = cj["u"].next()
                P.act(u.ap, Z1.ap, AF.Exp, reads=[Z1], writes=[u])
                st1[j][n] = u

            def S1b(j, n):
                cj = chain[j]
                kb = nk - 1 - n
                u = st1[j].pop(n)
                Lm = cj["Lm"].next()
                if kb >= nmask0:
                    Lt = cj["Lt"].next()
                    P.act(Lt.ap, u.ap, AF.Ln, reads=[u], writes=[Lt], bias=1.0, scale=1.0)
                    P.stt(Lm.ap, qpos_b.ap[:, qs_], kpos[:, kb:kb + 1], Lt.ap, ALU.is_gt, ALU.mult,
                          reads=[qpos_b, cst, Lt], writes=[Lm])
                else:
                    P.act(Lm.ap, u.ap, AF.Ln, reads=[u], writes=[Lm], bias=1.0, scale=1.0)
                st[j][n] = (Lm, kb)
                if n < nk - 1:
                    LSn = cj["LS"][(n + 1) % 3]
                    if n == 0:
                        P.copy(DVE, LSn.ap, Lm.ap, reads=[Lm], writes=[LSn])
                    else:
                        LSo = cj["LS"][n % 3]
                        P.tt(LSn.ap, LSo.ap, Lm.ap, ALU.add, reads=[LSo, Lm], writes=[LSn])

            def S2(j, n):
                cj = chain[j]
                Lm, kb = st[j].pop(n)
                Z2 = cj["Z2"]
                js = slice(j * 64, (j + 1) * 64)
                P.mm(Z2.ap, kt.ap[js, kb * 128:(kb + 1) * 128], QT_all.ap[js, p, qs_], True, False,
                     reads=[kt, QT_all], writes=[Z2])
                P.mm(Z2.ap, negtri_bf, Lm.ap, False, n == 0, reads=[cbf, Lm], writes=[Z2])
                if n > 0:
                    LSo = cj["LS"][n % 3]
                    P.mm(Z2.ap, negones128, LSo.ap, False, True, reads=[cbf, LSo], writes=[Z2])
                A = cj["A"].next()
                if kb >= nmask0:
                    At = cj["At"].next()
                    P.act(At.ap, Z2.ap, AF.Exp, reads=[Z2], writes=[At])
                    P.stt(A.ap, qpos_b.ap[:, qs_], kpos[:, kb:kb + 1], At.ap, ALU.is_gt, ALU.mult,
                          reads=[qpos_b, cst, At], writes=[A])
                else:
                    P.act(A.ap, Z2.ap, AF.Exp, reads=[Z2], writes=[A])
                st3[j][n] = (A, kb)

            def S3(j, n):
                cj = chain[j]
                A, kb = st3[j].pop(n)
                O = cj["O"]
                P.mm(O.ap, vp.ap[:, kb, :], A.ap, n == 0, n == nk - 1, reads=[vp, A], writes=[O])

            for n in range(nk + 2):
                if n < nk:
                    S1(0, n)
                    S1(1, n)
                    S1b(0, n)
                    S1b(1, n)
                if 1 <= n <= nk:
                    S2(0, n - 1)
                    S2(1, n - 1)
                if n >= 2:
                    S3(0, n - 2)
                    S3(1, n - 2)
            for j in range(2):
                O = chain[j]["O"]
                js = slice(j * 64, (j + 1) * 64)
                P.copy(DVE, ysbT_all.ap[js, p, qs_], O.ap[js, :], reads=[O], writes=[ysbT_all])
    if debug:
        finals.append(P.dma(SP, dbg["ysb"][:, :], ysbT_all.ap.rearrange("p a b -> p (a b)"), reads=[ysbT_all]))
    P.release(mB2)
    if stop_after == "B2":
        P.emit(final_waits=finals + list(P.prev_dmas) + list(P.dmas))
        return nc

    mB3 = P.mark()
    Wgs = P.alloc([8, D], BF16, name="Wgs")
    Wgh = P.alloc([8, D], BF16, name="Wgh")
    Wbs = P.alloc([4, D], BF16, name="Wbs")
    Wbh = P.alloc([4, D], BF16, name="Wbh")
    Wo = P.alloc([8, D], BF16, name="Wo")
    for hh in range(2):
        P.dma(POOL, Wgs.ap[:, :, hh * 512:(hh + 1) * 512], wslice(3584 + hh * 512, 512), writes=[Wgs])
        P.dma(POOL, Wgh.ap[:, :, hh * 512:(hh + 1) * 512], wslice(4608 + hh * 512, 512), writes=[Wgh])
    P.dma(POOL, Wbs.ap, w_bsb.rearrange("(c p) n -> p c n", p=128), writes=[Wbs])
    P.dma(POOL, Wbh.ap, w_bhg.rearrange("(c p) n -> p c n", p=128), writes=[Wbh])
    P.dma(POOL, Wo.ap, w_out.rearrange("(c p) n -> p c n", p=128), writes=[Wo])
    B3slots = [dict(xt=P.alloc([D], F32, name=f"xc{k}"), hb=P.alloc([D], BF16, name=f"hbc{k}"),
                    st=P.alloc([8], F32, name=f"stc{k}"), hT=P.alloc([8, 128], BF16, name=f"hTc{k}"),
                    sg=[P.alloc([512], F32, name=f"sig{k}{i}") for i in range(3)],
                    m=[P.alloc([512], F32, name=f"m32{k}{i}") for i in range(2)],
                    mg=P.alloc([D], BF16, name=f"mg{k}"), mT=P.alloc([8, 128], BF16, name=f"mT{k}"),
                    x1=P.alloc([D], F32, name=f"x1{k}"),
                    tp=P.psum[4 * k], ps=[P.psum[4 * k + 1], P.psum[4 * k + 2], P.psum[4 * k + 3]]) for k in range(2)]

    def blockB3(tb):
        def gen(sl):
            ts_ = slice(tb * 128, (tb + 1) * 128)
            xt, hb, s_, hT, mg, mT, x1 = sl["xt"], sl["hb"], sl["st"], sl["hT"], sl["mg"], sl["mT"], sl["x1"]
            s1, s2, tmp = sl["sg"]
            m1, m2 = sl["m"]
            tp = sl["tp"]
            pa, pb, pc = sl["ps"]
            tpv = tp.ap.bitcast(BF16).rearrange("p (a b) -> p a b", b=128)
            P.dma(SP, xt.ap, x_own[ts_, :], writes=[xt])
            yield
            P.memset(s_.ap[:, 0:1], 0.0, writes=[s_])
            P.act(junk.ap, xt.ap, AF.Square, reads=[xt, s_], writes=[junk, s_], accum_out=s_.ap[:, 0:1])
            P.act(s_.ap[:, 1:2], s_.ap[:, 0:1], AF.Ln, reads=[s_], writes=[s_], scale=1.0 / D, bias=eps_col)
            P.act(s_.ap[:, 2:3], s_.ap[:, 1:2], AF.Exp, reads=[s_], writes=[s_], scale=-0.5)
            yield
            P.stt(hb.ap, xt.ap, s_.ap[:, 2:3], ln1_b.ap, ALU.mult, ALU.mult, reads=[xt, s_, ln1_b], writes=[hb])
            yield
            for c in range(8):
                P.tr(tpv[:, c, :], hb.ap[:, c * 128:(c + 1) * 128], ident_bf, reads=[hb, cbf], writes=[tp])
            yield
            P.copy(ACT, hT.ap, tpv, reads=[tp], writes=[hT])
            yield
            for hh in range(2):
                cs = slice(hh * 512, (hh + 1) * 512)
                for c in range(8):
                    P.mm(pa.ap, hT.ap[:, c, :], Wgs.ap[:, c, cs], c == 0, c == 7, reads=[hT, Wgs], writes=[pa])
                for c in range(8):
                    P.mm(pb.ap, hT.ap[:, c, :], Wgh.ap[:, c, cs], c == 0, c == 7, reads=[hT, Wgh], writes=[pb])
                for c4 in range(4):
                    P.mm(pc.ap, ysbT_all.ap[:, c4, ts_], Wbs.ap[:, c4, cs], c4 == 0, c4 == 3, reads=[ysbT_all, Wbs], writes=[pc])
                yield
                sigmoid_from(pa.ap, pa, 128, s1, tmp)
                yield
                sigmoid_from(pb.ap, pb, 128, s2, tmp)
                P.tt(m1.ap, s1.ap, pc.ap, ALU.mult, reads=[s1, pc], writes=[m1])
                yield
                for c4 in range(4):
                    P.mm(pa.ap, yhgT_all.ap[:, c4, ts_], Wbh.ap[:, c4, cs], c4 == 0, c4 == 3, reads=[yhgT_all, Wbh], writes=[pa])
                yield
                P.tt(m2.ap, s2.ap, pa.ap, ALU.mult, reads=[s2, pa], writes=[m2])
                P.tt(mg.ap[:, cs], m1.ap, m2.ap, ALU.add, reads=[m1, m2], writes=[mg])
                yield
            for c in range(8):
                P.tr(tpv[:, c, :], mg.ap[:, c * 128:(c + 1) * 128], ident_bf, reads=[mg, cbf], writes=[tp])
            yield
            P.copy(ACT, mT.ap, tpv, reads=[tp], writes=[mT])
            yield
            for hh, po in ((0, pa), (1, pb)):
                cs = slice(hh * 512, (hh + 1) * 512)
                for c in range(8):
                    P.mm(po.ap, mT.ap[:, c, :], Wo.ap[:, c, cs], c == 0, c == 7, reads=[mT, Wo], writes=[po])
            yield
            for hh, po in ((0, pa), (1, pb)):
                cs = slice(hh * 512, (hh + 1) * 512)
                P.tt(x1.ap[:, cs], xt.ap[:, cs], po.ap, ALU.add, reads=[xt, po], writes=[x1])
            P.dma(POOL, x1_d[ts_, :], x1.ap, reads=[x1], writes=[bx1])
        return gen

    run_tasks([dict(kind="B", gen=blockB3(tb), deps=[], fin=None) for tb in range(16)], {"B": B3slots})
    P.release(glob_mark)
    if stop_after == "B3":
        P.emit(final_waits=finals + list(P.prev_dmas) + list(P.dmas))
        return nc

    ln2_b = P.alloc([D], F32, name="ln2_b")
    P.dma(SP, ln2_b.ap, ln2_d.partition_broadcast(128), writes=[ln2_b])
    fin_b = P.alloc([D], F32, name="fin_b")
    P.dma(SP, fin_b.ap, fin_d.partition_broadcast(128), writes=[fin_b])
    brt_b = P.alloc([20], F32, name="brt_b")
    P.dma(SP, brt_b.ap, b_rt.partition_broadcast(128), writes=[brt_b])
    Wr = P.alloc([8, 20], F32, name="Wr")
    P.dma(SP, Wr.ap, w_rt.rearrange("(c p) n -> p c n", p=128), writes=[Wr])
    xnT_all = P.alloc([8, 2048], BF16, name="xnT_all")
    yacc = [P.alloc([D], F32, name=f"yacc{tb}") for tb in range(16)]
    comb = P.alloc([16, 16], F32, name="comb")
    Wge = [P.alloc([8, DEX], BF16, name=f"Wge{i}") for i in range(2)]
    Wue = [P.alloc([8, DEX], BF16, name=f"Wue{i}") for i in range(2)]
    Wde = [P.alloc([4, D], BF16, name=f"Wde{i}") for i in range(2)]

    def load_expert(e):
        k = e % 2
        P.dma(POOL, Wge[k].ap, w_eg[e].rearrange("(c p) n -> p c n", p=128), writes=[Wge[k]])
        P.dma(POOL, Wue[k].ap, w_eu[e].rearrange("(c p) n -> p c n", p=128), writes=[Wue[k]])
        P.dma(POOL, Wde[k].ap, w_ed[e].rearrange("(c p) n -> p c n", p=128), writes=[Wde[k]])

    load_expert(0)
    mC1 = P.mark()
    Cslots = [dict(xn=P.alloc([D], F32, name=f"xn{k}"), xnT32=P.alloc([8, 128], F32, name=f"xnT32{k}"),
                   rt=P.alloc([96], F32, name=f"rt{k}"), st=P.alloc([8], F32, name=f"stp{k}"),
                   tp=[P.psum[2 * k], P.psum[2 * k + 1]]) for k in range(4)]

    def blockC(tb):
        def gen(sl):
            ts_ = slice(tb * 128, (tb + 1) * 128)
            ya = yacc[tb]
            xn, xnT32, rt, s_ = sl["xn"], sl["xnT32"], sl["rt"], sl["st"]
            P.dma(SP, ya.ap, x1_d[ts_, :], reads=[bx1], writes=[ya])
            yield
            P.memset(s_.ap[:, 0:1], 0.0, writes=[s_])
            P.act(junk.ap, ya.ap, AF.Square, reads=[ya, s_], writes=[junk, s_], accum_out=s_.ap[:, 0:1])
            P.act(s_.ap[:, 1:2], s_.ap[:, 0:1], AF.Ln, reads=[s_], writes=[s_], scale=1.0 / D, bias=eps_col)
            P.act(s_.ap[:, 2:3], s_.ap[:, 1:2], AF.Exp, reads=[s_], writes=[s_], scale=-0.5)
            yield
            P.stt(xn.ap, ya.ap, s_.ap[:, 2:3], ln2_b.ap, ALU.mult, ALU.mult, reads=[ya, s_, ln2_b], writes=[xn])
            yield
            for half in range(2):
                tp = sl["tp"][half]
                tpv = tp.ap.rearrange("p (a b) -> p a b", b=128)
                for c in range(4):
                    cc = half * 4 + c
                    P.tr(tpv[:, c, :], xn.ap[:, cc * 128:(cc + 1) * 128], ident_f, reads=[xn, cst], writes=[tp])
            yield
            for half in range(2):
                tp = sl["tp"][half]
                tpv = tp.ap.rearrange("p (a b) -> p a b", b=128)
                P.copy(ACT if half else DVE, xnT32.ap[:, half * 4:(half + 1) * 4, :], tpv, reads=[tp], writes=[xnT32])
            yield
            pr = sl["tp"][0]
            for c in range(8):
                P.mm(pr.ap[:, 0:20], xnT32.ap[:, c, :], Wr.ap[:, c, :], c == 0, c == 7, reads=[xnT32, Wr], writes=[pr])
            yield
            P.copy(ACT, xnT_all.ap[:, :, ts_], xnT32.ap, reads=[xnT32], writes=[xnT_all])
            R = rt.ap
            lg, gl, el = R[:, 0:20], R[:, 0:4], R[:, 4:20]
            gmax, ngmax, gsum, wgrp = R[:, 20:21], R[:, 21:22], R[:, 22:23], R[:, 23:24]
            goh, gpen, ge = R[:, 24:28], R[:, 28:32], R[:, 32:36]
            em, oh1, em2 = R[:, 36:52], R[:, 52:68], R[:, 68:84]
            P.tt(lg, pr.ap[:, 0:20], brt_b.ap, ALU.add, reads=[pr, brt_b], writes=[rt])
            yield
            P.reduce(gmax, gl, ALU.max, reads=[rt], writes=[rt])
            P.ts(goh, gl, gmax, None, ALU.is_equal, reads=[rt], writes=[rt])
            P.ts(ngmax, gmax, -1.0, None, ALU.mult, reads=[rt], writes=[rt])
            P.memset(gsum, 0.0, writes=[rt])
            P.act(ge, gl, AF.Exp, reads=[rt], writes=[rt], bias=ngmax, scale=1.0, accum_out=gsum)
            yield
            P.recip(wgrp, gsum, reads=[rt], writes=[rt])
            P.ts(gpen, goh, 1e30, -1e30, ALU.mult, ALU.add, reads=[rt], writes=[rt])
            P.tt(em.rearrange("p (g e) -> p g e", g=4), el.rearrange("p (g e) -> p g e", g=4),
                 gpen.unsqueeze(2).to_broadcast([128, 4, 4]), ALU.add, reads=[rt], writes=[rt])
            m1, m2, dd, ed, w1, w2 = R[:, 84:85], R[:, 85:86], R[:, 86:87], R[:, 87:88], R[:, 88:89], R[:, 89:90]
            P.reduce(m1, em, ALU.max, reads=[rt], writes=[rt])
            yield
            P.ts(oh1, em, m1, None, ALU.is_equal, reads=[rt], writes=[rt])
            P.stt(em2, oh1, -1e30, em, ALU.mult, ALU.add, reads=[rt], writes=[rt])
            P.reduce(m2, em2, ALU.max, reads=[rt], writes=[rt])
            P.tt(dd, m2, m1, ALU.subtract, reads=[rt], writes=[rt])
            P.act(ed, dd, AF.Exp, reads=[rt], writes=[rt])
            yield
            P.ts(w1, ed, 1.0, None, ALU.add, reads=[rt], writes=[rt])
            P.recip(w1, w1, reads=[rt], writes=[rt])
            P.tt(w2, ed, w1, ALU.mult, reads=[rt], writes=[rt])
            P.tt(w1, w1, wgrp, ALU.mult, reads=[rt], writes=[rt])
            P.tt(w2, w2, wgrp, ALU.mult, reads=[rt], writes=[rt])
            yield
            P.ts(em, em2, m2, None, ALU.is_equal, reads=[rt], writes=[rt])
            P.ts(oh1, oh1, w1, None, ALU.mult, reads=[rt], writes=[rt])
            P.stt(comb.ap[:, tb, :], em, w2, oh1, ALU.mult, ALU.add, reads=[rt], writes=[comb])
        return gen

    run_tasks([dict(kind="C", gen=blockC(tb), deps=[], fin=None) for tb in range(16)], {"C": Cslots})
    P.release(mC1)
    sgr = Ring([P.alloc([512], F32, name=f"sge{i}") for i in range(4)])
    aTr = Ring([P.alloc([4, 512], BF16, name=f"aT{i}") for i in range(2)])
    outr = Ring([P.alloc([D], F32, name=f"ob{i}") for i in range(2)])

    def final_norm(tb):
        ya = yacc[tb]
        s = rms_stats(ya.ap, ya, D)
        ob = outr.next()
        P.stt(ob.ap, ya.ap, s.ap[:, 2:3], fin_b.ap, ALU.mult, ALU.mult, reads=[ya, s, fin_b], writes=[ob])
        finals.append(P.dma(POOL, out_d[tb * 128:(tb + 1) * 128, :], ob.ap, reads=[ob]))

    for e in range(NE):
        if e + 1 < NE:
            load_expert(e + 1)
        k = e % 2
        wg, wu, wd = Wge[k], Wue[k], Wde[k]
        for tt_ in range(4):
            tks = slice(tt_ * 512, (tt_ + 1) * 512)
            aT = aTr.next()
            for hc in range(4):
                hs = slice(hc * 128, (hc + 1) * 128)
                pg, pu = ps_next(0, 8), ps_next(0, 8)
                for c in range(8):
                    P.mm(pg.ap, wg.ap[:, c, hs], xnT_all.ap[:, c, tks], c == 0, c == 7, reads=[wg, xnT_all], writes=[pg])
                for c in range(8):
                    P.mm(pu.ap, wu.ap[:, c, hs], xnT_all.ap[:, c, tks], c == 0, c == 7, reads=[wu, xnT_all], writes=[pu])
                t_ = sgr.next()
                P.act(t_.ap, pg.ap, AF.Silu, reads=[pg], writes=[t_])
                P.tt(aT.ap[:, hc, :], t_.ap, pu.ap, ALU.mult, reads=[t_, pu], writes=[aT])
            for blk in range(4):
                tb = tt_ * 4 + blk
                for hh in range(2):
                    cs = slice(hh * 512, (hh + 1) * 512)
                    py = ps_next(0, 8)
                    for hc in range(4):
                        P.mm(py.ap, aT.ap[:, hc, blk * 128:(blk + 1) * 128], wd.ap[:, hc, cs], hc == 0, hc == 3,
                             reads=[aT, wd], writes=[py])
                    ya = yacc[tb]
                    P.stt(ya.ap[:, cs], py.ap, comb.ap[:, tb, e:e + 1], ya.ap[:, cs], ALU.mult, ALU.add,
                          reads=[py, comb, ya], writes=[ya])
                if e == NE - 1:
                    final_norm(tb)
    P.emit(final_waits=finals)
    return nc


def core_tiles(r):
    return [r, 7 - r, 8 + r, 15 - r]


def make_in_maps(inputs):
    f = lambda a: np.ascontiguousarray(np.asarray(a, dtype=np.float32))
    x = f(inputs["x"])
    consts = make_consts()
    w_rt = np.ascontiguousarray(np.concatenate([f(inputs["w_router_group"])[0], f(inputs["w_router_expert"])[0]], axis=1))
    b_rt = np.ascontiguousarray(np.concatenate([f(inputs["b_router_group"])[0], f(inputs["b_router_expert"])[0]], axis=0))
    shared = {
        "consts": consts,
        "w_in": f(inputs["w_in"])[0], "w_bsb": f(inputs["w_branch_sb"])[0], "w_bhg": f(inputs["w_branch_hg"])[0],
        "w_out": f(inputs["w_out"])[0], "w_rt": w_rt, "b_rt": b_rt,
        "w_eg": f(inputs["w_exp_gate"])[0], "w_eu": f(inputs["w_exp_up"])[0], "w_ed": f(inputs["w_exp_down"])[0],
        "ln1_g": f(inputs["ln1_g"])[0], "ln2_g": f(inputs["ln2_g"])[0], "final_g": f(inputs["final_g"]),
        "hg_norm_g": f(inputs["hg_norm_g"])[0], "hg_lb_logits": f(inputs["hg_lb_logits"]),
    }
    maps = []
    for c in range(8):
        b, r = c // 4, c % 4
        tiles = core_tiles(r)
        x_own = np.ascontiguousarray(np.concatenate([x[b, t * 512:(t + 1) * 512] for t in tiles], axis=0))
        pos = np.concatenate([np.arange(t * 512, (t + 1) * 512) for t in tiles]).astype(np.float32)
        qpos = np.ascontiguousarray(np.broadcast_to(pos[None, :], (128, 2048)))
        sel = np.zeros((64, 64), np.float32)
        for i, t in enumerate(tiles):
            sel[:, i * 16 + t] = 1.0
        m = dict(shared)
        m.update({"x_all": np.ascontiguousarray(x[b]), "x_own": x_own, "qpos": qpos, "sel": sel})
        maps.append(m)
    return maps


_NC_CACHE = {}


def kernel(**inputs):
    if "nc" not in _NC_CACHE:
        _NC_CACHE["nc"] = build_program()
    nc = _NC_CACHE["nc"]
    maps = make_in_maps(inputs)
    res = run_bass_kernel_spmd(nc, maps, core_ids=list(range(8)))
    x = np.asarray(inputs["x"])
    out = np.zeros(x.shape, np.float32)
    for c in range(8):
        b, r = c // 4, c % 4
        o = np.asarray(res.results[c]["out"])
        for i, t in enumerate(core_tiles(r)):
            out[b, t * 512:(t + 1) * 512] = o[i * 512:(i + 1) * 512]
    return out
```

```python
import contextlib
import numpy as np
import concourse.bass as bass
import concourse.mybir as mybir
from concourse.bass_utils import run_bass_kernel_spmd

F32 = mybir.dt.float32
BF16 = mybir.dt.bfloat16
U8 = mybir.dt.uint8
AF = mybir.ActivationFunctionType
ALU = mybir.AluOpType
AX = mybir.AxisListType
PE, ACT, DVE, POOL, SP = "tensor", "scalar", "vector", "gpsimd", "sync"
ENGS = (PE, ACT, DVE, POOL, SP)
SEM_LIMIT = 30000
EPS = 1e-6
ARENA = 207 * 1024
DSZ = {F32: 4, BF16: 2, U8: 1}

D = 1024
NE = 16
DEX = 512


class Buf:
    __slots__ = ("name", "w_eng", "w_dma", "r_eng", "r_dma")

    def __init__(self, name=""):
        self.name = name
        self.w_eng = {}
        self.w_dma = []
        self.r_eng = {}
        self.r_dma = []


class Tile:
    __slots__ = ("ap", "b")

    def __init__(self, ap, b):
        self.ap = ap
        self.b = b


class Op:
    __slots__ = ("eng", "fn", "deps", "signal", "ticket", "is_dma", "prev")

    def __init__(self, eng, fn, is_dma):
        self.eng = eng
        self.fn = fn
        self.deps = []
        self.signal = is_dma
        self.ticket = None
        self.prev = None
        self.is_dma = is_dma


def _b(x):
    return x.b if isinstance(x, Tile) else x


class Prog:
    def __init__(self, nc):
        self.nc = nc
        self.ops = {e: [] for e in ENGS}
        self.stack = contextlib.ExitStack()
        self.arena = self.stack.enter_context(nc.sbuf_tensor("arena", [128, ARENA], U8))
        self.off = 0
        self.peak = 0
        self.last = {e: None for e in ENGS}
        self.dmas = []
        self.prev_dmas = []
        self.pending = {e: [] for e in ENGS}
        self.psum = []
        for i in range(8):
            t = self.stack.enter_context(nc.psum_tensor(f"ps{i}", [128, 512], F32))
            self.psum.append(Tile(t[:], Buf(f"ps{i}")))

    def alloc(self, shape, dtype, parts=128, name=""):
        n = 1
        for s in shape:
            n *= s
        nbytes = n * DSZ[dtype]
        off = (self.off + 63) // 64 * 64
        assert off + nbytes <= ARENA, f"arena overflow {name} {off + nbytes}"
        self.off = off + nbytes
        self.peak = max(self.peak, self.off)
        ap = self.arena[0:parts, off:off + nbytes].bitcast(dtype)
        if len(shape) == 2:
            ap = ap.rearrange("p (a b) -> p a b", a=shape[0])
        elif len(shape) == 3:
            ap = ap.rearrange("p (a b c) -> p a b c", a=shape[0], b=shape[1])
        return Tile(ap, Buf(name))

    def mark(self):
        return self.off

    def release(self, m):
        self.off = m
        self.barrier()

    def barrier(self):
        lasts = [o for o in self.last.values() if o is not None]
        for e in ENGS:
            self.pending[e] = list(lasts) + list(self.dmas)
        self.prev_dmas = list(self.dmas)
        self.dmas = []

    def op(self, eng, fn, reads=(), writes=(), is_dma=False):
        o = Op(eng, fn, is_dma)
        deps = []
        same_ok = not is_dma
        for x in reads:
            b = _b(x)
            deps.extend(b.w_eng.values())
            deps.extend(b.w_dma)
        for x in writes:
            b = _b(x)
            for d in list(b.w_eng.values()) + list(b.r_eng.values()):
                deps.append(d)
            deps.extend(b.w_dma)
            deps.extend(b.r_dma)
        deps.extend(self.pending[eng])
        self.pending[eng] = []
        seen = set()
        for d in deps:
            if d is o or id(d) in seen:
                continue
            seen.add(id(d))
            if d.eng == PE and eng == PE and not d.is_dma and not is_dma:
                continue
            o.deps.append(d)
            d.signal = True
        for x in reads:
            b = _b(x)
            if is_dma:
                b.r_dma.append(o)
            else:
                b.r_eng[eng] = o
        for x in writes:
            b = _b(x)
            if b.r_eng or b.r_dma:
                b.w_eng = {}
                b.w_dma = []
                b.r_eng = {}
                b.r_dma = []
            if is_dma:
                b.w_dma.append(o)
            else:
                b.w_eng[eng] = o
        self.ops[eng].append(o)
        self.last[eng] = o
        if is_dma:
            self.dmas.append(o)
        return o

    def dma(self, eng, out, in_, reads=(), writes=()):
        return self.op(eng, lambda e: e.dma_start(out=out, in_=in_), reads, writes, is_dma=True)

    def mm(self, out, lhsT, rhs, start, stop, reads=(), writes=()):
        return self.op(PE, lambda e: e.matmul(out, lhsT=lhsT, rhs=rhs, start=start, stop=stop), reads, writes)

    def tr(self, out, in_, ident, reads=(), writes=()):
        return self.op(PE, lambda e: e.transpose(out, in_, ident), reads, writes)

    def act(self, out, in_, func, reads=(), writes=(), **kw):
        return self.op(ACT, lambda e: e.activation(out=out, in_=in_, func=func, **kw), reads, writes)

    def copy(self, eng, out, in_, reads=(), writes=()):
        if eng == ACT:
            return self.op(ACT, lambda e: e.activation(out=out, in_=in_, func=AF.Copy), reads, writes)
        return self.op(eng, lambda e: e.tensor_copy(out=out, in_=in_), reads, writes)

    def tt(self, out, in0, in1, op, reads=(), writes=(), eng=DVE):
        return self.op(eng, lambda e: e.tensor_tensor(out=out, in0=in0, in1=in1, op=op), reads, writes)

    def ts(self, out, in0, s1, s2, op0, op1=None, reads=(), writes=(), eng=DVE):
        if op1 is None:
            return self.op(eng, lambda e: e.tensor_scalar(out=out, in0=in0, scalar1=s1, scalar2=None, op0=op0), reads, writes)
        return self.op(eng, lambda e: e.tensor_scalar(out=out, in0=in0, scalar1=s1, scalar2=s2, op0=op0, op1=op1), reads, writes)

    def stt(self, out, in0, scalar, in1, op0, op1, reads=(), writes=(), eng=DVE):
        return self.op(eng, lambda e: e.scalar_tensor_tensor(out=out, in0=in0, scalar=scalar, in1=in1, op0=op0, op1=op1), reads, writes)

    def recip(self, out, in_, reads=(), writes=()):
        return self.op(DVE, lambda e: e.reciprocal(out=out, in_=in_), reads, writes)

    def memset(self, out, val, writes=(), eng=DVE):
        return self.op(eng, lambda e: e.memset(out, val), (), writes)

    def reduce(self, out, in_, op, reads=(), writes=()):
        return self.op(DVE, lambda e: e.tensor_reduce(out=out, in_=in_, axis=AX.X, op=op), reads, writes)

    def emit(self, final_waits=()):
        nc = self.nc
        st = self.stack
        for o in final_waits:
            o.signal = True
        nsem = [0]

        def newsem(nm):
            nsem[0] += 1
            return st.enter_context(nc.semaphore(f"{nm}{nsem[0]}"))

        for e, lst in self.ops.items():
            cur, cnt = None, 0
            pool, pcnt, k = [], [], 0
            for o in lst:
                if not o.signal:
                    continue
                if o.is_dma:
                    if len(pool) < 16:
                        pool.append(newsem("d" + e[:2]))
                        pcnt.append(0)
                    j = k % len(pool)
                    k += 1
                    if pcnt[j] + 16 > SEM_LIMIT:
                        pool[j] = newsem("d" + e[:2])
                        pcnt[j] = 0
                    if pcnt[j] > 0:
                        o.prev = (pool[j], pcnt[j])
                    pcnt[j] += 16
                    o.ticket = (pool[j], pcnt[j])
                else:
                    if cur is None or cnt >= SEM_LIMIT:
                        cur = newsem("c" + e[:2])
                        cnt = 0
                    cnt += 1
                    o.ticket = (cur, cnt)
        self.nsem = nsem[0]
        finals = list(final_waits)

        with nc.Block() as block:
            def make(ename):
                lst = self.ops[ename]

                def body(eng):
                    waited = {}
                    for o in lst:
                        need = {}
                        tl = [d.ticket for d in o.deps]
                        if o.prev is not None:
                            tl.append(o.prev)
                        for sem, val in tl:
                            k = id(sem)
                            if k not in need or need[k][1] < val:
                                need[k] = (sem, val)
                        for k, (sem, val) in need.items():
                            if waited.get(k, 0) >= val:
                                continue
                            waited[k] = val
                            eng.wait_ge(sem, val)
                        ins = o.fn(eng)
                        if o.signal:
                            ins.then_inc(o.ticket[0], 16 if o.is_dma else 1)
                    if ename == SP:
                        for d in finals:
                            eng.wait_ge(d.ticket[0], d.ticket[1])
                return body

            block.sync(make(SP))
            block.tensor(make(PE))
            block.scalar(make(ACT))
            block.vector(make(DVE))
            block.gpsimd(make(POOL))
        st.close()


class Ring:
    def __init__(self, tiles):
        self.tiles = tiles
        self.i = 0

    def next(self):
        t = self.tiles[self.i % len(self.tiles)]
        self.i += 1
        return t


def run_tasks(tasks, pools):
    free = {k: list(v) for k, v in pools.items()}
    done = [False] * len(tasks)
    active = []
    nxt = 0
    while nxt < len(tasks) or active:
        while nxt < len(tasks):
            t = tasks[nxt]
            if not all(done[d] for d in t["deps"]) or not free[t["kind"]]:
                break
            sl = free[t["kind"]].pop(0)
            active.append((nxt, t["gen"](sl), sl))
            nxt += 1
        assert active, "task deadlock"
        keep = []
        for idx, g, sl in active:
            try:
                next(g)
                keep.append((idx, g, sl))
            except StopIteration:
                done[idx] = True
                if tasks[idx].get("fin"):
                    tasks[idx]["fin"](sl)
                free[tasks[idx]["kind"]].append(sl)
        active = keep


C_ID = 0
C_NTRI = 128
C_KPOS = 256
C_TRI64 = 320
C_SUP64 = 384
C_SUP128 = 448
NCONST = 576


def make_consts():
    c = np.zeros((128, NCONST), np.float32)
    j = np.arange(128)
    c[:, C_ID:C_ID + 128] = np.eye(128)
    c[:, C_NTRI:C_NTRI + 128] = -(j[:, None] >= j[None, :]).astype(np.float32)
    c[:, C_KPOS:C_KPOS + 64] = np.arange(64)[None, :] * 128 + j[:, None]
    s = np.arange(64)
    c[:64, C_TRI64:C_TRI64 + 64] = (s[:, None] <= s[None, :])
    c[:64, C_SUP64:C_SUP64 + 64] = (s[:, None] > s[None, :])
    c[:, C_SUP128:C_SUP128 + 128] = (j[:, None] > j[None, :])
    return c


def build_program(stop_after=None, debug=False, nT=16, nslots=4, b1_level=2, sim_softplus=False):
    nc = bass.Bass("TRN2", target_bir_lowering=False)
    P = Prog(nc)

    def din(n, s):
        return nc.dram_tensor(n, list(s), F32, kind="ExternalInput").ap()

    x_all = din("x_all", [8192, D])
    x_own = din("x_own", [2048, D])
    qpos_d = din("qpos", [128, 2048])
    sel_d = din("sel", [64, 64])
    consts_d = din("consts", [128, NCONST])
    w_in = din("w_in", [D, 5632])
    w_bsb = din("w_bsb", [512, D])
    w_bhg = din("w_bhg", [512, D])
    w_out = din("w_out", [D, D])
    w_rt = din("w_rt", [D, 20])
    b_rt = din("b_rt", [20])
    w_eg = din("w_eg", [NE, D, DEX])
    w_eu = din("w_eu", [NE, D, DEX])
    w_ed = din("w_ed", [NE, DEX, D])
    ln1_d = din("ln1_g", [D])
    ln2_d = din("ln2_g", [D])
    fin_d = din("final_g", [D])
    hgn_d = din("hg_norm_g", [512])
    lbl_d = din("hg_lb_logits", [2, 512])
    out_d = nc.dram_tensor("out", [2048, D], F32, kind="ExternalOutput").ap()
    kd_kind = "ExternalOutput" if debug else "Internal"
    KT_d = nc.dram_tensor("KT_d", [4, 128, 8192], BF16, kind=kd_kind).ap()
    V_d = nc.dram_tensor("V_d", [4, 64, 128, 128], BF16, kind=kd_kind).ap()
    x1_d = nc.dram_tensor("x1_d", [2048, D], F32, kind=kd_kind).ap()
    dbg = {}
    if debug:
        dbg["ssel"] = nc.dram_tensor("dbg_ssel", [64, 4 * 512], F32, kind="ExternalOutput").ap()
        dbg["qt"] = nc.dram_tensor("dbg_qt", [128, 4 * 2048], BF16, kind="ExternalOutput").ap()
        dbg["yhg"] = nc.dram_tensor("dbg_yhg", [128, 4 * 2048], BF16, kind="ExternalOutput").ap()
        dbg["ysb"] = nc.dram_tensor("dbg_ysb", [128, 4 * 2048], BF16, kind="ExternalOutput").ap()
    bKT, bV, bx1 = Buf("KT_d"), Buf("V_d"), Buf("x1_d")
    finals = []

    def wslice(c0, n):
        return w_in[:, c0:c0 + n].rearrange("(c p) n -> p c n", p=128)

    cst = P.alloc([NCONST], F32, name="cst")
    P.dma(SP, cst.ap, consts_d[:, :], writes=[cst])
    cbf = P.alloc([384], BF16, name="cbf")
    P.dma(POOL, cbf.ap[:, 0:256], consts_d[:, 0:256], writes=[cbf])
    P.memset(cbf.ap[:, 256:384], -1.0, writes=[cbf])
    negones128 = cbf.ap[:, 256:384]
    ident_bf = cbf.ap[:, 0:128]
    negtri_bf = cbf.ap[:, 128:256]
    ident_f = cst.ap[:, C_ID:C_ID + 128]
    small = P.alloc([16], F32, name="small")
    P.memset(small.ap[:, 0:1], EPS, writes=[small])
    P.memset(small.ap[:, 1:2], 1.0, writes=[small])
    eps_col = small.ap[:, 0:1]
    ones_f = small.ap[:, 1:2]
    smallb = P.alloc([132], BF16, name="smallb")
    P.memset(smallb.ap[:, 0:1], 1.0, writes=[smallb])
    P.memset(smallb.ap[:, 4:132], -1.0, writes=[smallb])
    ones_bf_col = smallb.ap[:, 0:1]
    negones_row = smallb.ap[0:1, 4:132]
    junk = P.alloc([D], BF16, name="junk")
    stat = Ring([P.alloc([8], F32, name=f"stat{i}") for i in range(4)])
    glob_mark = P.mark()
    ln1_b = P.alloc([D], F32, name="ln1_b")
    P.dma(SP, ln1_b.ap, ln1_d.partition_broadcast(128), writes=[ln1_b])
    hgn_b = P.alloc([512], F32, name="hgn_b")
    P.dma(SP, hgn_b.ap, hgn_d.partition_broadcast(128), writes=[hgn_b])
    lb_b = P.alloc([512], F32, name="lb_b")
    oml_b = P.alloc([512], F32, name="oml_b")
    Ssel = P.alloc([4, 512], F32, parts=64, name="Ssel")
    P.memset(Ssel.ap, 0.0, writes=[Ssel])
    sel_t = P.alloc([64], F32, parts=64, name="sel")
    P.dma(SP, sel_t.ap, sel_d[:, :], writes=[sel_t])
    Wf = P.alloc([8, 512], BF16, name="Wf")
    Wi = P.alloc([8, 512], BF16, name="Wi")
    P.dma(POOL, Wf.ap, w_in[:, 2048:2560].rearrange("(c p) n -> p c n", p=128), writes=[Wf])
    P.dma(POOL, Wi.ap, w_in[:, 2560:3072].rearrange("(c p) n -> p c n", p=128), writes=[Wi])
    base_mark = P.mark()
    lbt = P.alloc([2, 512], F32, name="lbt")
    P.dma(SP, lbt.ap[:, 0, :], lbl_d[0, :].partition_broadcast(128), writes=[lbt])
    P.dma(SP, lbt.ap[:, 1, :], lbl_d[1, :].partition_broadcast(128), writes=[lbt])
    tmpl = P.alloc([512], F32, name="tmpl")
    P.tt(tmpl.ap, lbt.ap[:, 1, :], lbt.ap[:, 0, :], ALU.subtract, reads=[lbt], writes=[tmpl])
    P.act(tmpl.ap, tmpl.ap, AF.Exp, reads=[tmpl], writes=[tmpl])
    P.ts(tmpl.ap, tmpl.ap, 1.0, None, ALU.add, reads=[tmpl], writes=[tmpl])
    P.recip(lb_b.ap, tmpl.ap, reads=[tmpl], writes=[lb_b])
    P.ts(oml_b.ap, lb_b.ap, -1.0, 1.0, ALU.mult, ALU.add, reads=[lb_b], writes=[oml_b])

    psr = {"i": 0}

    def ps_next(lo=2, hi=8):
        k = lo + psr["i"] % (hi - lo)
        psr["i"] += 1
        return P.psum[k]

    tpr = {"i": 0}

    def tp_next():
        k = tpr["i"] % 2
        tpr["i"] += 1
        return P.psum[k]

    def rms_stats(src_ap, src_t, n, parts=128):
        s = stat.next()
        sa = s.ap[0:parts]
        P.memset(sa[:, 0:1], 0.0, writes=[s])
        P.act(junk.ap[0:parts, 0:n], src_ap, AF.Square, reads=[src_t, s], writes=[junk, s], accum_out=sa[:, 0:1])
        P.act(sa[:, 1:2], sa[:, 0:1], AF.Ln, reads=[s], writes=[s], scale=1.0 / n, bias=eps_col[0:parts])
        P.act(sa[:, 2:3], sa[:, 1:2], AF.Exp, reads=[s], writes=[s], scale=-0.5)
        return s

    def norm_transpose(xt, g_b, hb, hT_dst, hT_t):
        s = rms_stats(xt.ap, xt, D)
        P.stt(hb.ap, xt.ap, s.ap[:, 2:3], g_b.ap, ALU.mult, ALU.mult, reads=[xt, s, g_b], writes=[hb])
        tp = tp_next()
        tpv = tp.ap.bitcast(BF16).rearrange("p (a b) -> p a b", b=128)
        for c in range(8):
            P.tr(tpv[:, c, :], hb.ap[:, c * 128:(c + 1) * 128], ident_bf, reads=[hb, cbf], writes=[tp])
        P.copy(ACT, hT_dst, tpv, reads=[tp], writes=[hT_t])

    def proj_tok(ps, hT_ap, hT_t, tok0, ntok, W, reads_extra=()):
        for c in range(8):
            P.mm(ps.ap[0:ntok, :], hT_ap[:, c, tok0:tok0 + ntok], W.ap[:, c, :], c == 0, c == 7,
                 reads=[hT_t, W], writes=[ps])

    def sigmoid_from(ps_ap, ps_t, parts, out_t, tmp_t):
        P.act(tmp_t.ap[0:parts], ps_ap, AF.Exp, reads=[ps_t], writes=[tmp_t], scale=-1.0)
        P.act(tmp_t.ap[0:parts], tmp_t.ap[0:parts], AF.Ln, reads=[tmp_t], writes=[tmp_t], bias=1.0, scale=1.0)
        P.act(out_t.ap[0:parts], tmp_t.ap[0:parts], AF.Exp, reads=[tmp_t], writes=[out_t], scale=-1.0)

    mA = base_mark
    Wk = P.alloc([8, 512], BF16, name="Wk")
    Wv = P.alloc([8, 512], BF16, name="Wv")
    P.dma(POOL, Wk.ap, wslice(512, 512), writes=[Wk])
    P.dma(POOL, Wv.ap, wslice(1024, 512), writes=[Wv])
    NHT = 3
    hT_tiles = [P.alloc([8, 512], BF16, name=f"hT{i}") for i in range(NHT)]
    S = P.alloc([8, 64], F32, parts=64, name="S")
    P.memset(S.ap, 0.0, writes=[S])
    sup128 = cst.ap[:, C_SUP128:C_SUP128 + 128]
    Fslots = [dict(x=[P.alloc([D], F32, name=f"xa{k}{i}") for i in range(4)],
                   hb=[P.alloc([D], BF16, name=f"hba{k}{i}") for i in range(2)],
                   st=[P.alloc([8], F32, name=f"sta{k}{i}") for i in range(2)],
                   KTt=P.alloc([4, 512], BF16, name=f"KTt{k}"), Vt=P.alloc([4, 512], BF16, name=f"Vt{k}"),
                   tp=P.psum[k], ps=P.psum[2 + k]) for k in range(2)]
    Hslots = [dict(f32=[P.alloc([512], F32, name=f"fa{k}{i}") for i in range(4)],
                   bf=[P.alloc([512], BF16, name=f"ba{k}{i}") for i in range(2)],
                   et=P.alloc([8], F32, parts=64, name=f"et{k}"),
                   usb=P.alloc([512], F32, parts=64, name=f"us{k}"), ps=P.psum[4 + k]) for k in range(4)]

    def ssel_acc(T):
        for i in range(4):
            col = i * 16 + T
            P.stt(Ssel.ap[:, i, :], S.ap.rearrange("p h d -> p (h d)"), sel_t.ap[:, col:col + 1], Ssel.ap[:, i, :],
                  ALU.mult, ALU.add, reads=[S, sel_t, Ssel], writes=[Ssel])

    def frontA(T):
        def gen(sl):
            hT = hT_tiles[T % NHT]
            xts = sl["x"]
            tp, ps = sl["tp"], sl["ps"]
            tpv = tp.ap.bitcast(BF16).rearrange("p (a b) -> p a b", b=128)
            for blk in range(4):
                r0 = T * 512 + blk * 128
                P.dma(SP, xts[blk].ap, x_all[r0:r0 + 128, :], writes=[xts[blk]])
            yield
            for blk in range(4):
                xt, hb, s_ = xts[blk], sl["hb"][blk % 2], sl["st"][blk % 2]
                P.memset(s_.ap[:, 0:1], 0.0, writes=[s_])
                P.act(junk.ap, xt.ap, AF.Square, reads=[xt, s_], writes=[junk, s_], accum_out=s_.ap[:, 0:1])
                P.act(s_.ap[:, 1:2], s_.ap[:, 0:1], AF.Ln, reads=[s_], writes=[s_], scale=1.0 / D, bias=eps_col)
                P.act(s_.ap[:, 2:3], s_.ap[:, 1:2], AF.Exp, reads=[s_], writes=[s_], scale=-0.5)
                yield
                P.stt(hb.ap, xt.ap, s_.ap[:, 2:3], ln1_b.ap, ALU.mult, ALU.mult, reads=[xt, s_, ln1_b], writes=[hb])
                yield
                for c in range(8):
                    P.tr(tpv[:, c, :], hb.ap[:, c * 128:(c + 1) * 128], ident_bf, reads=[hb, cbf], writes=[tp])
                yield
                P.copy(ACT if blk % 2 else DVE, hT.ap[:, :, blk * 128:(blk + 1) * 128], tpv, reads=[tp], writes=[hT])
                yield
            KTt = sl["KTt"]
            for p in range(4):
                for c in range(8):
                    P.mm(ps.ap, Wk.ap[:, c, p * 128:(p + 1) * 128], hT.ap[:, c, :], c == 0, c == 7,
                         reads=[Wk, hT], writes=[ps])
                yield
                P.copy(ACT if p % 2 else DVE, KTt.ap[:, p, :], ps.ap, reads=[ps], writes=[KTt])
                yield
            P.dma(POOL, KT_d[:, :, T * 512:(T + 1) * 512].rearrange("q p t -> p q t"), KTt.ap, reads=[KTt], writes=[bKT])
            Vt = sl["Vt"]
            for blk in range(4):
                proj_tok(ps, hT.ap, hT, blk * 128, 128, Wv)
                yield
                P.copy(DVE if blk % 2 else ACT, Vt.ap[:, blk, :], ps.ap, reads=[ps], writes=[Vt])
                yield
            for blk in range(4):
                P.dma(POOL, V_d[:, T * 4 + blk, :, :].rearrange("p k c -> k p c"),
                      Vt.ap[:, blk, :].rearrange("k (p c) -> k p c", p=4), reads=[Vt], writes=[bV])
        return gen

    def hgA(T, blk):
        def gen(sl):
            hT = hT_tiles[T % NHT]
            sg, tmp, f, g = sl["f32"]
            ibf, kd = sl["bf"]
            et, usb, ps = sl["et"], sl["usb"], sl["ps"]
            proj_tok(ps, hT.ap, hT, blk * 128, 128, Wf)
            yield
            sigmoid_from(ps.ap, ps, 128, sg, tmp)
            yield
            proj_tok(ps, hT.ap, hT, blk * 128, 128, Wi)
            yield
            P.copy(DVE, ibf.ap, ps.ap, reads=[ps], writes=[ibf])
            P.tt(f.ap, sg.ap, oml_b.ap, ALU.mult, reads=[sg, oml_b], writes=[f])
            P.tt(f.ap, f.ap, lb_b.ap, ALU.add, reads=[f, lb_b], writes=[f])
            yield
            P.act(g.ap, f.ap, AF.Ln, reads=[f], writes=[g])
            kk = sg
            P.ts(kk.ap, f.ap, -1.0, 1.0, ALU.mult, ALU.add, reads=[f], writes=[kk])
            yield
            P.mm(ps.ap, sup128, g.ap, True, True, reads=[cst, g], writes=[ps])
            yield
            ed = tmp
            P.act(ed.ap, ps.ap, AF.Exp, reads=[ps], writes=[ed])
            yield
            for h in range(8):
                P.mm(ps.ap[0:64, h:h + 1], g.ap[:, h * 64:(h + 1) * 64], ones_f, True, True,
                     reads=[g, small], writes=[ps])
            yield
            P.act(et.ap, ps.ap[0:64, 0:8], AF.Exp, reads=[ps], writes=[et])
            P.tt(kd.ap, kk.ap, ed.ap, ALU.mult, reads=[kk, ed], writes=[kd])
            yield
            for h in range(8):
                P.mm(ps.ap[0:64, h * 64:(h + 1) * 64], kd.ap[:, h * 64:(h + 1) * 64], ibf.ap[:, h * 64:(h + 1) * 64],
                     True, True, reads=[kd, ibf], writes=[ps])
            yield
            P.copy(ACT, usb.ap, ps.ap[0:64, :], reads=[ps], writes=[usb])

        def fin(sl):
            et, usb = sl["et"], sl["usb"]
            if blk == 0:
                ssel_acc(T)
            P.tt(S.ap, S.ap, et.ap.unsqueeze(2).to_broadcast([64, 8, 64]), ALU.mult, reads=[S, et], writes=[S])
            P.tt(S.ap, S.ap, usb.ap.rearrange("p (h d) -> p h d", h=8), ALU.add, reads=[S, usb], writes=[S])
        return gen, fin

    tasksA = []
    fidx = {}
    hidx = {}

    def addF(T):
        fidx[T] = len(tasksA)
        tasksA.append(dict(kind="F", gen=frontA(T), deps=list(hidx.get(T - NHT, [])), fin=None))

    addF(0)
    for T in range(nT):
        if T + 1 < nT:
            addF(T + 1)
        if T < nT - 1:
            hidx[T] = []
            for blk in range(4):
                g_, f_ = hgA(T, blk)
                hidx[T].append(len(tasksA))
                tasksA.append(dict(kind="H", gen=g_, deps=[fidx[T]], fin=f_))
    run_tasks(tasksA, {"F": Fslots, "H": Hslots})
    ssel_acc(nT - 1)
    if debug:
        finals.append(P.dma(SP, dbg["ssel"][:, :], Ssel.ap.rearrange("p a b -> p (a b)"), reads=[Ssel]))
    P.release(mA)
    if stop_after == "A":
        P.emit(final_waits=finals + list(P.prev_dmas) + list(P.dmas))
        return nc

    QT_all = P.alloc([4, 2048], BF16, name="QT_all")
    yhgT_all = P.alloc([4, 2048], BF16, name="yhgT_all")
    mB1 = P.mark()
    Wq = P.alloc([8, 512], BF16, name="Wq")
    Wqh = P.alloc([8, 512], BF16, name="Wqh")
    Wg = P.alloc([8, 512], BF16, name="Wg")
    P.dma(POOL, Wq.ap, wslice(0, 512), writes=[Wq])
    P.dma(POOL, Wqh.ap, wslice(1536, 512), writes=[Wqh])
    P.dma(POOL, Wg.ap, wslice(3072, 512), writes=[Wg])
    hT_tiles = [P.alloc([8, 512], BF16, name=f"hTb{i}") for i in range(2)]
    FBslots = [dict(x=[P.alloc([D], F32, name=f"xb{i}") for i in range(4)],
                    hb=[P.alloc([D], BF16, name=f"hbb{i}") for i in range(1)] * 2,
                    st=[P.alloc([8], F32, name=f"stb{i}") for i in range(2)],
                    tp=P.psum[0], ps=P.psum[1])]
    CHslots = [dict(f32=[P.alloc([512], F32, parts=64, name=f"fb{k}{i}") for i in range(7)],
                    qd=P.alloc([512], BF16, parts=64, name=f"qd{k}"), kdn=P.alloc([512], BF16, parts=64, name=f"kdn{k}"),
                    kT=P.alloc([8, 64], BF16, parts=64, name=f"kT{k}"),
                    qdT=P.alloc([8, 64], BF16, parts=64, name=f"qdT{k}"), scT=P.alloc([8, 64], BF16, parts=64, name=f"scT{k}"),
                    ibf=P.alloc([512], BF16, parts=64, name=f"ibf{k}"), kd=P.alloc([512], BF16, parts=64, name=f"kd{k}"),
                    sgt=P.alloc([512], BF16, parts=64, name=f"sgt{k}"), ybf=P.alloc([512], BF16, parts=64, name=f"ybf{k}"),
                    et=P.alloc([8], F32, parts=64, name=f"etc{k}"), s8=P.alloc([24], F32, parts=64, name=f"s8{k}"),
                    ps=P.psum[2 + 2 * k], tp=P.psum[3 + 2 * k])
               for k in range(3)]
    S_t = [P.alloc([8, 64], F32, parts=64, name=f"S1{i}") for i in range(2)]
    Sbf_t = [P.alloc([8, 64], BF16, parts=64, name=f"Sbf{i}") for i in range(2)]
    tri64 = cst.ap[0:64, C_TRI64:C_TRI64 + 64]
    sup64 = cst.ap[0:64, C_SUP64:C_SUP64 + 64]
    id64 = ident_bf[0:64, 0:64]
    turn = {"n": 0}

    def frontB(i):
        def gen(sl):
            hT = hT_tiles[i % 2]
            xts = sl["x"]
            tp, ps = sl["tp"], sl["ps"]
            tpv = tp.ap.bitcast(BF16).rearrange("p (a b) -> p a b", b=128)
            for blk in range(4):
                r0 = i * 512 + blk * 128
                P.dma(SP, xts[blk].ap, x_own[r0:r0 + 128, :], writes=[xts[blk]])
            yield
            for blk in range(4):
                xt, hb, s_ = xts[blk], sl["hb"][blk % 2], sl["st"][blk % 2]
                P.memset(s_.ap[:, 0:1], 0.0, writes=[s_])
                P.act(junk.ap, xt.ap, AF.Square, reads=[xt, s_], writes=[junk, s_], accum_out=s_.ap[:, 0:1])
                P.act(s_.ap[:, 1:2], s_.ap[:, 0:1], AF.Ln, reads=[s_], writes=[s_], scale=1.0 / D, bias=eps_col)
                P.act(s_.ap[:, 2:3], s_.ap[:, 1:2], AF.Exp, reads=[s_], writes=[s_], scale=-0.5)
                yield
                P.stt(hb.ap, xt.ap, s_.ap[:, 2:3], ln1_b.ap, ALU.mult, ALU.mult, reads=[xt, s_, ln1_b], writes=[hb])
                yield
                for c in range(8):
                    P.tr(tpv[:, c, :], hb.ap[:, c * 128:(c + 1) * 128], ident_bf, reads=[hb, cbf], writes=[tp])
                yield
                P.copy(ACT if blk % 2 else DVE, hT.ap[:, :, blk * 128:(blk + 1) * 128], tpv, reads=[tp], writes=[hT])
                yield
            for p in range(4):
                for c in range(8):
                    P.mm(ps.ap, Wq.ap[:, c, p * 128:(p + 1) * 128], hT.ap[:, c, :], c == 0, c == 7,
                         reads=[Wq, hT], writes=[ps])
                yield
                P.act(QT_all.ap[:, p, i * 512:(i + 1) * 512], ps.ap, AF.Copy, reads=[ps], writes=[QT_all], scale=0.125)
                yield
        return gen

    def chunkB(i, ch):
        myturn = i * 8 + ch

        def gen(sl):
            hT = hT_tiles[i % 2]
            S, Sbf_r, Sbf_w = S_t[i % 2], Sbf_t[ch % 2], Sbf_t[(ch + 1) % 2]
            t0 = ch * 64
            t_sg, t_tmp, t_f, t_g, t_rq, t_qs, t_er = sl["f32"]
            t_us = t_er
            qd, kdn, kT, qdT, scT, ibf, kd, sgt, ybf, et, s8 = (
                sl[k] for k in ("qd", "kdn", "kT", "qdT", "scT", "ibf", "kd", "sgt", "ybf", "et", "s8"))
            ps, tp = sl["ps"], sl["tp"]
            P64 = ps.ap[0:64, :]
            proj_tok(ps, hT.ap, hT, t0, 64, Wf)
            yield
            sigmoid_from(P64, ps, 64, t_sg, t_tmp)
            yield
            P.tt(t_f.ap, t_sg.ap, oml_b.ap[0:64], ALU.mult, reads=[t_sg, oml_b], writes=[t_f])
            P.tt(t_f.ap, t_f.ap, lb_b.ap[0:64], ALU.add, reads=[t_f, lb_b], writes=[t_f])
            kk = t_sg
            P.ts(kk.ap, t_f.ap, -1.0, 1.0, ALU.mult, ALU.add, reads=[t_f], writes=[kk])
            proj_tok(ps, hT.ap, hT, t0, 64, Wqh)
            yield
            P.act(t_g.ap, t_f.ap, AF.Ln, reads=[t_f], writes=[t_g])
            sigmoid_from(P64, ps, 64, t_rq, t_tmp)
            yield
            P.tt(t_qs.ap, t_rq.ap, P64, ALU.mult, reads=[t_rq, ps], writes=[t_qs])
            yield
            proj_tok(ps, hT.ap, hT, t0, 64, Wg)
            yield
            sigmoid_from(P64, ps, 64, t_rq, t_tmp)
            yield
            P.tt(sgt.ap, t_rq.ap, P64, ALU.mult, reads=[t_rq, ps], writes=[sgt])
            yield
            proj_tok(ps, hT.ap, hT, t0, 64, Wi)
            yield
            P.copy(ACT, ibf.ap, P64, reads=[ps], writes=[ibf])
            yield
            ecum, encum, erev = t_rq, t_tmp, t_er
            P.mm(P64, tri64, t_g.ap, True, True, reads=[cst, t_g], writes=[ps])
            yield
            P.act(ecum.ap, P64, AF.Exp, reads=[ps], writes=[ecum])
            P.act(encum.ap, P64, AF.Exp, reads=[ps], writes=[encum], scale=-1.0)
            yield
            P.mm(P64, sup64, t_g.ap, True, True, reads=[cst, t_g], writes=[ps])
            P.tt(qd.ap, t_qs.ap, ecum.ap, ALU.mult, reads=[t_qs, ecum], writes=[qd])
            P.tt(kdn.ap, kk.ap, encum.ap, ALU.mult, reads=[kk, encum], writes=[kdn])
            yield
            P.act(erev.ap, P64, AF.Exp, reads=[ps], writes=[erev])
            yield
            for h in range(8):
                P.mm(ps.ap[0:64, h:h + 1], t_g.ap[:, h * 64:(h + 1) * 64], ones_f[0:64], True, True,
                     reads=[t_g, small], writes=[ps])
            P.tt(kd.ap, kk.ap, erev.ap, ALU.mult, reads=[kk, erev], writes=[kd])
            tqv = tp.ap.bitcast(BF16)[0:64, 0:512].rearrange("p (h t) -> p h t", h=8)
            for h in range(8):
                P.tr(tqv[:, h, :], qd.ap[:, h * 64:(h + 1) * 64], id64, reads=[qd, cbf], writes=[tp])
            yield
            P.act(et.ap, ps.ap[0:64, 0:8], AF.Exp, reads=[ps], writes=[et])
            P.copy(ACT, qdT.ap, tqv, reads=[tp], writes=[qdT])
            yield
            for h in range(8):
                P.mm(ps.ap[0:64, h * 64:(h + 1) * 64], kd.ap[:, h * 64:(h + 1) * 64], ibf.ap[:, h * 64:(h + 1) * 64],
                     True, True, reads=[kd, ibf], writes=[ps])
            for h in range(8):
                P.tr(tqv[:, h, :], kdn.ap[:, h * 64:(h + 1) * 64], id64, reads=[kdn, cbf], writes=[tp])
            yield
            P.copy(ACT, t_us.ap, P64, reads=[ps], writes=[t_us])
            P.copy(DVE, kT.ap, tqv, reads=[tp], writes=[kT])
            yield
            for h in range(8):
                P.mm(ps.ap[0:64, h * 64:(h + 1) * 64], kT.ap[:, h, :], qdT.ap[:, h, :], True, True,
                     reads=[kT, qdT], writes=[ps])
            yield
            P.tt(scT.ap, P64.rearrange("p (h t) -> p h t", h=8),
                 tri64.unsqueeze(1).to_broadcast([64, 8, 64]), ALU.mult, reads=[ps, cst], writes=[scT])
            yield
            while turn["n"] != myturn:
                yield
            if ch == 0:
                P.copy(DVE, S.ap.rearrange("p h d -> p (h d)"), Ssel.ap[:, i, :], reads=[Ssel], writes=[S])
                P.copy(DVE, Sbf_r.ap, S.ap, reads=[S], writes=[Sbf_r])
            for h in range(8):
                hs = slice(h * 64, (h + 1) * 64)
                P.mm(ps.ap[0:64, hs], scT.ap[:, h, :], ibf.ap[:, hs], True, False, reads=[scT, ibf], writes=[ps])
                P.mm(ps.ap[0:64, hs], qdT.ap[:, h, :], Sbf_r.ap[:, h, :], False, True, reads=[qdT, Sbf_r], writes=[ps])
            if ch < 7:
                P.tt(S.ap, S.ap, et.ap.unsqueeze(2).to_broadcast([64, 8, 64]), ALU.mult, reads=[S, et], writes=[S])
                P.tt(S.ap, S.ap, t_us.ap.rearrange("p (h d) -> p h d", h=8), ALU.add, reads=[S, t_us], writes=[S])
                P.copy(DVE, Sbf_w.ap, S.ap, reads=[S], writes=[Sbf_w])
            turn["n"] += 1
            yield
            osb, sq, y1 = t_f, t_g, t_qs
            P.copy(ACT, osb.ap, P64, reads=[ps], writes=[osb])
            yield
            P.tt(sq.ap, osb.ap, osb.ap, ALU.mult, reads=[osb], writes=[sq])
            P.reduce(s8.ap[:, 0:8], sq.ap.rearrange("p (h d) -> p h d", h=8), ALU.add, reads=[sq], writes=[s8])
            yield
            P.act(s8.ap[:, 8:16], s8.ap[:, 0:8], AF.Ln, reads=[s8], writes=[s8], scale=1.0 / 64, bias=eps_col[0:64])
            P.act(s8.ap[:, 16:24], s8.ap[:, 8:16], AF.Exp, reads=[s8], writes=[s8], scale=-0.5)
            yield
            P.tt(y1.ap.rearrange("p (h d) -> p h d", h=8), osb.ap.rearrange("p (h d) -> p h d", h=8),
                 s8.ap[:, 16:24].unsqueeze(2).to_broadcast([64, 8, 64]), ALU.mult, reads=[osb, s8], writes=[y1])
            P.tt(y1.ap, y1.ap, hgn_b.ap[0:64], ALU.mult, reads=[y1, hgn_b], writes=[y1])
            P.tt(ybf.ap, y1.ap, sgt.ap, ALU.mult, reads=[y1, sgt], writes=[ybf])
            yield
            tyv = tp.ap.bitcast(BF16)[:, 0:256].rearrange("p (c t) -> p c t", c=4)
            for c4 in range(4):
                P.tr(tyv[:, c4, :], ybf.ap[:, c4 * 128:(c4 + 1) * 128], id64, reads=[ybf, cbf], writes=[tp])
            yield
            tok = i * 512 + ch * 64
            P.copy(ACT, yhgT_all.ap[:, :, tok:tok + 64], tyv, reads=[tp], writes=[yhgT_all])
        return gen, None

    tasksB = []
    fb = {}
    cb = {}

    def addFB(i):
        fb[i] = len(tasksB)
        tasksB.append(dict(kind="F", gen=frontB(i), deps=list(cb.get(i - 2, [])), fin=None))

    addFB(0)
    for i in range(nslots):
        if i + 1 < nslots:
            addFB(i + 1)
        cb[i] = []
        if b1_level >= 1:
            for ch in range(8):
                g_, f_ = chunkB(i, ch)
                cb[i].append(len(tasksB))
                tasksB.append(dict(kind="CH", gen=g_, deps=[fb[i]], fin=f_))
    run_tasks(tasksB, {"F": FBslots, "CH": CHslots})
    if debug:
        finals.append(P.dma(SP, dbg["qt"][:, :], QT_all.ap.rearrange("p a b -> p (a b)"), reads=[QT_all]))
        finals.append(P.dma(SP, dbg["yhg"][:, :], yhgT_all.ap.rearrange("p a b -> p (a b)"), reads=[yhgT_all]))
    P.release(mB1)
    if stop_after == "B1":
        P.emit(final_waits=finals + list(P.prev_dmas) + list(P.dmas))
        return nc

    ysbT_all = P.alloc([4, 2048], BF16, name="ysbT_all")
    mB2 = P.mark()
    qpos_b = P.alloc([2048], F32, name="qpos_b")
    P.dma(SP, qpos_b.ap, qpos_d[:, :], writes=[qpos_b])
    KTp = [P.alloc([8192], BF16, name=f"KTp{i}") for i in range(2)]
    Vp = [P.alloc([64, 128], BF16, name=f"Vp{i}") for i in range(2)]
    kpos = cst.ap[:, C_KPOS:C_KPOS + 64]
    chain = []
    for j in range(2):
        chain.append(dict(
            u=Ring([P.alloc([512], BF16, name=f"u{j}{k}") for k in range(2)]),
            Lt=Ring([P.alloc([512], BF16, name=f"Lt{j}{k}") for k in range(2)]),
            Lm=Ring([P.alloc([512], BF16, name=f"Lm{j}{k}") for k in range(3)]),
            At=Ring([P.alloc([512], BF16, name=f"At{j}{k}") for k in range(2)]),
            A=Ring([P.alloc([512], BF16, name=f"A{j}{k}") for k in range(3)]),
            LS=[P.alloc([512], BF16, name=f"LS{j}{k}") for k in range(3)],
            Z1=[P.psum[2 * j], P.psum[6 + j]],
            Z2=P.psum[2 * j + 1],
            O=P.psum[4 + j],
        ))

    def load_pair(p):
        kt, vp = KTp[p % 2], Vp[p % 2]
        P.dma(SP, kt.ap, KT_d[p], reads=[bKT], writes=[kt])
        for q in range(4):
            P.dma(SP, vp.ap[:, q * 16:(q + 1) * 16, :], V_d[p, q * 16:(q + 1) * 16, :, :].rearrange("b k c -> k b c"),
                  reads=[bV], writes=[vp])

    load_pair(0)
    for p in range(4):
        if p + 1 < 4:
            load_pair(p + 1)
        kt, vp = KTp[p % 2], Vp[p % 2]
        for i in range(4):
            nk = 16 * (i + 1)
            nmask0 = 16 * i
            qs_ = slice(i * 512, (i + 1) * 512)
            st = [dict(), dict()]
            st3 = [dict(), dict()]
            st1 = [dict(), dict()]

            def S1(j, n):
                cj = chain[j]
                kb = nk - 1 - n
                Z1 = cj["Z1"][n % 2]
                js = slice(j * 64, (j + 1) * 64)
                P.mm(Z1.ap, kt.ap[js, kb * 128:(kb + 1) * 128], QT_all.ap[js, p, qs_], True, True,
                     reads=[kt, QT_all], writes=[Z1])
                u = cj["u"].next()
                P.act(u.ap, Z1.ap, AF.Exp, reads=[Z1], writes=[u])
                st1[j][n] = u

            def S1b(j, n):
                cj = chain[j]
                kb = nk - 1 - n
                u = st1[j].pop(n)
                Lm = cj["Lm"].next()
                if kb >= nmask0:
                    Lt = cj["Lt"].next()
                    P.act(Lt.ap, u.ap, AF.Ln, reads=[u], writes=[Lt], bias=1.0, scale=1.0)
                    P.stt(Lm.ap, qpos_b.ap[:, qs_], kpos[:, kb:kb + 1], Lt.ap, ALU.is_gt, ALU.mult,
                          reads=[qpos_b, cst, Lt], writes=[Lm])
                else:
                    P.act(Lm.ap, u.ap, AF.Ln, reads=[u], writes=[Lm], bias=1.0, scale=1.0)
                st[j][n] = (Lm, kb)
                if n < nk - 1:
                    LSn = cj["LS"][(n + 1) % 3]
                    if n == 0:
                        P.copy(DVE, LSn.ap, Lm.ap, reads=[Lm], writes=[LSn])
                    else:
                        LSo = cj["LS"][n % 3]
                        P.tt(LSn.ap, LSo.ap, Lm.ap, ALU.add, reads=[LSo, Lm], writes=[LSn])

            def S2(j, n):
                cj = chain[j]
                Lm, kb = st[j].pop(n)
                Z2 = cj["Z2"]
                js = slice(j * 64, (j + 1) * 64)
                P.mm(Z2.ap, kt.ap[js, kb * 128:(kb + 1) * 128], QT_all.ap[js, p, qs_], True, False,
                     reads=[kt, QT_all], writes=[Z2])
                P.mm(Z2.ap, negtri_bf, Lm.ap, False, n == 0, reads=[cbf, Lm], writes=[Z2])
                if n > 0:
                    LSo = cj["LS"][n % 3]
                    P.mm(Z2.ap, negones128, LSo.ap, False, True, reads=[cbf, LSo], writes=[Z2])
                A = cj["A"].next()
                if kb >= nmask0:
                    At = cj["At"].next()
                    P.act(At.ap, Z2.ap, AF.Exp, reads=[Z2], writes=[At])
                    P.stt(A.ap, qpos_b.ap[:, qs_], kpos[:, kb:kb + 1], At.ap, ALU.is_gt, ALU.mult,
                          reads=[qpos_b, cst, At], writes=[A])
                else:
                    P.act(A.ap, Z2.ap, AF.Exp, reads=[Z2], writes=[A])
                st3[j][n] = (A, kb)

            def S3(j, n):
                cj = chain[j]
                A, kb = st3[j].pop(n)
                O = cj["O"]
                P.mm(O.ap, vp.ap[:, kb, :], A.ap, n == 0, n == nk - 1, reads=[vp, A], writes=[O])

            for n in range(nk + 2):
                if n < nk:
                    S1(0, n)
                    S1(1, n)
                    S1b(0, n)
                    S1b(1, n)
                if 1 <= n <= nk:
                    S2(0, n - 1)
                    S2(1, n - 1)
                if n >= 2:
                    S3(0, n - 2)
                    S3(1, n - 2)
            for j in range(2):
                O = chain[j]["O"]
                js = slice(j * 64, (j + 1) * 64)
                P.copy(DVE, ysbT_all.ap[js, p, qs_], O.ap[js, :], reads=[O], writes=[ysbT_all])
    if debug:
        finals.append(P.dma(SP, dbg["ysb"][:, :], ysbT_all.ap.rearrange("p a b -> p (a b)"), reads=[ysbT_all]))
    P.release(mB2)
    if stop_after == "B2":
        P.emit(final_waits=finals + list(P.prev_dmas) + list(P.dmas))
        return nc

    mB3 = P.mark()
    Wgs = P.alloc([8, D], BF16, name="Wgs")
    Wgh = P.alloc([8, D], BF16, name="Wgh")
    Wbs = P.alloc([4, D], BF16, name="Wbs")
    Wbh = P.alloc([4, D], BF16, name="Wbh")
    Wo = P.alloc([8, D], BF16, name="Wo")
    for hh in range(2):
        P.dma(POOL, Wgs.ap[:, :, hh * 512:(hh + 1) * 512], wslice(3584 + hh * 512, 512), writes=[Wgs])
        P.dma(POOL, Wgh.ap[:, :, hh * 512:(hh + 1) * 512], wslice(4608 + hh * 512, 512), writes=[Wgh])
    P.dma(POOL, Wbs.ap, w_bsb.rearrange("(c p) n -> p c n", p=128), writes=[Wbs])
    P.dma(POOL, Wbh.ap, w_bhg.rearrange("(c p) n -> p c n", p=128), writes=[Wbh])
    P.dma(POOL, Wo.ap, w_out.rearrange("(c p) n -> p c n", p=128), writes=[Wo])
    B3slots = [dict(xt=P.alloc([D], F32, name=f"xc{k}"), hb=P.alloc([D], BF16, name=f"hbc{k}"),
                    st=P.alloc([8], F32, name=f"stc{k}"), hT=P.alloc([8, 128], BF16, name=f"hTc{k}"),
                    sg=[P.alloc([512], F32, name=f"sig{k}{i}") for i in range(3)],
                    m=[P.alloc([512], F32, name=f"m32{k}{i}") for i in range(2)],
                    mg=P.alloc([D], BF16, name=f"mg{k}"), mT=P.alloc([8, 128], BF16, name=f"mT{k}"),
                    x1=P.alloc([D], F32, name=f"x1{k}"),
                    tp=P.psum[4 * k], ps=[P.psum[4 * k + 1], P.psum[4 * k + 2], P.psum[4 * k + 3]]) for k in range(2)]

    def blockB3(tb):
        def gen(sl):
            ts_ = slice(tb * 128, (tb + 1) * 128)
            xt, hb, s_, hT, mg, mT, x1 = sl["xt"], sl["hb"], sl["st"], sl["hT"], sl["mg"], sl["mT"], sl["x1"]
            s1, s2, tmp = sl["sg"]
            m1, m2 = sl["m"]
            tp = sl["tp"]
            pa, pb, pc = sl["ps"]
            tpv = tp.ap.bitcast(BF16).rearrange("p (a b) -> p a b", b=128)
            P.dma(SP, xt.ap, x_own[ts_, :], writes=[xt])
            yield
            P.memset(s_.ap[:, 0:1], 0.0, writes=[s_])
            P.act(junk.ap, xt.ap, AF.Square, reads=[xt, s_], writes=[junk, s_], accum_out=s_.ap[:, 0:1])
            P.act(s_.ap[:, 1:2], s_.ap[:, 0:1], AF.Ln, reads=[s_], writes=[s_], scale=1.0 / D, bias=eps_col)
            P.act(s_.ap[:, 2:3], s_.ap[:, 1:2], AF.Exp, reads=[s_], writes=[s_], scale=-0.5)
            yield
            P.stt(hb.ap, xt.ap, s_.ap[:, 2:3], ln1_b.ap, ALU.mult, ALU.mult, reads=[xt, s_, ln1_b], writes=[hb])
            yield
            for c in range(8):
                P.tr(tpv[:, c, :], hb.ap[:, c * 128:(c + 1) * 128], ident_bf, reads=[hb, cbf], writes=[tp])
            yield
            P.copy(ACT, hT.ap, tpv, reads=[tp], writes=[hT])
            yield
            for hh in range(2):
                cs = slice(hh * 512, (hh + 1) * 512)
                for c in range(8):
                    P.mm(pa.ap, hT.ap[:, c, :], Wgs.ap[:, c, cs], c == 0, c == 7, reads=[hT, Wgs], writes=[pa])
                for c in range(8):
                    P.mm(pb.ap, hT.ap[:, c, :], Wgh.ap[:, c, cs], c == 0, c == 7, reads=[hT, Wgh], writes=[pb])
                for c4 in range(4):
                    P.mm(pc.ap, ysbT_all.ap[:, c4, ts_], Wbs.ap[:, c4, cs], c4 == 0, c4 == 3, reads=[ysbT_all, Wbs], writes=[pc])
                yield
                sigmoid_from(pa.ap, pa, 128, s1, tmp)
                yield
                sigmoid_from(pb.ap, pb, 128, s2, tmp)
                P.tt(m1.ap, s1.ap, pc.ap, ALU.mult, reads=[s1, pc], writes=[m1])
                yield
                for c4 in range(4):
                    P.mm(pa.ap, yhgT_all.ap[:, c4, ts_], Wbh.ap[:, c4, cs], c4 == 0, c4 == 3, reads=[yhgT_all, Wbh], writes=[pa])
                yield
                P.tt(m2.ap, s2.ap, pa.ap, ALU.mult, reads=[s2, pa], writes=[m2])
                P.tt(mg.ap[:, cs], m1.ap, m2.ap, ALU.add, reads=[m1, m2], writes=[mg])
                yield
            for c in range(8):
                P.tr(tpv[:, c, :], mg.ap[:, c * 128:(c + 1) * 128], ident_bf, reads=[mg, cbf], writes=[tp])
            yield
            P.copy(ACT, mT.ap, tpv, reads=[tp], writes=[mT])
            yield
            for hh, po in ((0, pa), (1, pb)):
                cs = slice(hh * 512, (hh + 1) * 512)
                for c in range(8):
                    P.mm(po.ap, mT.ap[:, c, :], Wo.ap[:, c, cs], c == 0, c == 7, reads=[mT, Wo], writes=[po])
            yield
            for hh, po in ((0, pa), (1, pb)):
                cs = slice(hh * 512, (hh + 1) * 512)
                P.tt(x1.ap[:, cs], xt.ap[:, cs], po.ap, ALU.add, reads=[xt, po], writes=[x1])
            P.dma(POOL, x1_d[ts_, :], x1.ap, reads=[x1], writes=[bx1])
        return gen

    run_tasks([dict(kind="B", gen=blockB3(tb), deps=[], fin=None) for tb in range(16)], {"B": B3slots})
    P.release(glob_mark)
    if stop_after == "B3":
        P.emit(final_waits=finals + list(P.prev_dmas) + list(P.dmas))
        return nc

    ln2_b = P.alloc([D], F32, name="ln2_b")
    P.dma(SP, ln2_b.ap, ln2_d.partition_broadcast(128), writes=[ln2_b])
    fin_b = P.alloc([D], F32, name="fin_b")
    P.dma(SP, fin_b.ap, fin_d.partition_broadcast(128), writes=[fin_b])
    brt_b = P.alloc([20], F32, name="brt_b")
    P.dma(SP, brt_b.ap, b_rt.partition_broadcast(128), writes=[brt_b])
    Wr = P.alloc([8, 20], F32, name="Wr")
    P.dma(SP, Wr.ap, w_rt.rearrange("(c p) n -> p c n", p=128), writes=[Wr])
    xnT_all = P.alloc([8, 2048], BF16, name="xnT_all")
    yacc = [P.alloc([D], F32, name=f"yacc{tb}") for tb in range(16)]
    comb = P.alloc([16, 16], F32, name="comb")
    Wge = [P.alloc([8, DEX], BF16, name=f"Wge{i}") for i in range(2)]
    Wue = [P.alloc([8, DEX], BF16, name=f"Wue{i}") for i in range(2)]
    Wde = [P.alloc([4, D], BF16, name=f"Wde{i}") for i in range(2)]

    def load_expert(e):
        k = e % 2
        P.dma(POOL, Wge[k].ap, w_eg[e].rearrange("(c p) n -> p c n", p=128), writes=[Wge[k]])
        P.dma(POOL, Wue[k].ap, w_eu[e].rearrange("(c p) n -> p c n", p=128), writes=[Wue[k]])
        P.dma(POOL, Wde[k].ap, w_ed[e].rearrange("(c p) n -> p c n", p=128), writes=[Wde[k]])

    load_expert(0)
    mC1 = P.mark()
    Cslots = [dict(xn=P.alloc([D], F32, name=f"xn{k}"), xnT32=P.alloc([8, 128], F32, name=f"xnT32{k}"),
                   rt=P.alloc([96], F32, name=f"rt{k}"), st=P.alloc([8], F32, name=f"stp{k}"),
                   tp=[P.psum[2 * k], P.psum[2 * k + 1]]) for k in range(4)]

    def blockC(tb):
        def gen(sl):
            ts_ = slice(tb * 128, (tb + 1) * 128)
            ya = yacc[tb]
            xn, xnT32, rt, s_ = sl["xn"], sl["xnT32"], sl["rt"], sl["st"]
            P.dma(SP, ya.ap, x1_d[ts_, :], reads=[bx1], writes=[ya])
            yield
            P.memset(s_.ap[:, 0:1], 0.0, writes=[s_])
            P.act(junk.ap, ya.ap, AF.Square, reads=[ya, s_], writes=[junk, s_], accum_out=s_.ap[:, 0:1])
            P.act(s_.ap[:, 1:2], s_.ap[:, 0:1], AF.Ln, reads=[s_], writes=[s_], scale=1.0 / D, bias=eps_col)
            P.act(s_.ap[:, 2:3], s_.ap[:, 1:2], AF.Exp, reads=[s_], writes=[s_], scale=-0.5)
            yield
            P.stt(xn.ap, ya.ap, s_.ap[:, 2:3], ln2_b.ap, ALU.mult, ALU.mult, reads=[ya, s_, ln2_b], writes=[xn])
            yield
            for half in range(2):
                tp = sl["tp"][half]
                tpv = tp.ap.rearrange("p (a b) -> p a b", b=128)
                for c in range(4):
                    cc = half * 4 + c
                    P.tr(tpv[:, c, :], xn.ap[:, cc * 128:(cc + 1) * 128], ident_f, reads=[xn, cst], writes=[tp])
            yield
            for half in range(2):
                tp = sl["tp"][half]
                tpv = tp.ap.rearrange("p (a b) -> p a b", b=128)
                P.copy(ACT if half else DVE, xnT32.ap[:, half * 4:(half + 1) * 4, :], tpv, reads=[tp], writes=[xnT32])
            yield
            pr = sl["tp"][0]
            for c in range(8):
                P.mm(pr.ap[:, 0:20], xnT32.ap[:, c, :], Wr.ap[:, c, :], c == 0, c == 7, reads=[xnT32, Wr], writes=[pr])
            yield
            P.copy(ACT, xnT_all.ap[:, :, ts_], xnT32.ap, reads=[xnT32], writes=[xnT_all])
            R = rt.ap
            lg, gl, el = R[:, 0:20], R[:, 0:4], R[:, 4:20]
            gmax, ngmax, gsum, wgrp = R[:, 20:21], R[:, 21:22], R[:, 22:23], R[:, 23:24]
            goh, gpen, ge = R[:, 24:28], R[:, 28:32], R[:, 32:36]
            em, oh1, em2 = R[:, 36:52], R[:, 52:68], R[:, 68:84]
            P.tt(lg, pr.ap[:, 0:20], brt_b.ap, ALU.add, reads=[pr, brt_b], writes=[rt])
            yield
            P.reduce(gmax, gl, ALU.max, reads=[rt], writes=[rt])
            P.ts(goh, gl, gmax, None, ALU.is_equal, reads=[rt], writes=[rt])
            P.ts(ngmax, gmax, -1.0, None, ALU.mult, reads=[rt], writes=[rt])
            P.memset(gsum, 0.0, writes=[rt])
            P.act(ge, gl, AF.Exp, reads=[rt], writes=[rt], bias=ngmax, scale=1.0, accum_out=gsum)
            yield
            P.recip(wgrp, gsum, reads=[rt], writes=[rt])
            P.ts(gpen, goh, 1e30, -1e30, ALU.mult, ALU.add, reads=[rt], writes=[rt])
            P.tt(em.rearrange("p (g e) -> p g e", g=4), el.rearrange("p (g e) -> p g e", g=4),
                 gpen.unsqueeze(2).to_broadcast([128, 4, 4]), ALU.add, reads=[rt], writes=[rt])
            m1, m2, dd, ed, w1, w2 = R[:, 84:85], R[:, 85:86], R[:, 86:87], R[:, 87:88], R[:, 88:89], R[:, 89:90]
            P.reduce(m1, em, ALU.max, reads=[rt], writes=[rt])
            yield
            P.ts(oh1, em, m1, None, ALU.is_equal, reads=[rt], writes=[rt])
            P.stt(em2, oh1, -1e30, em, ALU.mult, ALU.add, reads=[rt], writes=[rt])
            P.reduce(m2, em2, ALU.max, reads=[rt], writes=[rt])
            P.tt(dd, m2, m1, ALU.subtract, reads=[rt], writes=[rt])
            P.act(ed, dd, AF.Exp, reads=[rt], writes=[rt])
            yield
            P.ts(w1, ed, 1.0, None, ALU.add, reads=[rt], writes=[rt])
            P.recip(w1, w1, reads=[rt], writes=[rt])
            P.tt(w2, ed, w1, ALU.mult, reads=[rt], writes=[rt])
            P.tt(w1, w1, wgrp, ALU.mult, reads=[rt], writes=[rt])
            P.tt(w2, w2, wgrp, ALU.mult, reads=[rt], writes=[rt])
            yield
            P.ts(em, em2, m2, None, ALU.is_equal, reads=[rt], writes=[rt])
            P.ts(oh1, oh1, w1, None, ALU.mult, reads=[rt], writes=[rt])
            P.stt(comb.ap[:, tb, :], em, w2, oh1, ALU.mult, ALU.add, reads=[rt], writes=[comb])
        return gen

    run_tasks([dict(kind="C", gen=blockC(tb), deps=[], fin=None) for tb in range(16)], {"C": Cslots})
    P.release(mC1)
    sgr = Ring([P.alloc([512], F32, name=f"sge{i}") for i in range(4)])
    aTr = Ring([P.alloc([4, 512], BF16, name=f"aT{i}") for i in range(2)])
    outr = Ring([P.alloc([D], F32, name=f"ob{i}") for i in range(2)])

    def final_norm(tb):
        ya = yacc[tb]
        s = rms_stats(ya.ap, ya, D)
        ob = outr.next()
        P.stt(ob.ap, ya.ap, s.ap[:, 2:3], fin_b.ap, ALU.mult, ALU.mult, reads=[ya, s, fin_b], writes=[ob])
        finals.append(P.dma(POOL, out_d[tb * 128:(tb + 1) * 128, :], ob.ap, reads=[ob]))

    for e in range(NE):
        if e + 1 < NE:
            load_expert(e + 1)
        k = e % 2
        wg, wu, wd = Wge[k], Wue[k], Wde[k]
        for tt_ in range(4):
            tks = slice(tt_ * 512, (tt_ + 1) * 512)
            aT = aTr.next()
            for hc in range(4):
                hs = slice(hc * 128, (hc + 1) * 128)
                pg, pu = ps_next(0, 8), ps_next(0, 8)
                for c in range(8):
                    P.mm(pg.ap, wg.ap[:, c, hs], xnT_all.ap[:, c, tks], c == 0, c == 7, reads=[wg, xnT_all], writes=[pg])
                for c in range(8):
                    P.mm(pu.ap, wu.ap[:, c, hs], xnT_all.ap[:, c, tks], c == 0, c == 7, reads=[wu, xnT_all], writes=[pu])
                t_ = sgr.next()
                P.act(t_.ap, pg.ap, AF.Silu, reads=[pg], writes=[t_])
                P.tt(aT.ap[:, hc, :], t_.ap, pu.ap, ALU.mult, reads=[t_, pu], writes=[aT])
            for blk in range(4):
                tb = tt_ * 4 + blk
                for hh in range(2):
                    cs = slice(hh * 512, (hh + 1) * 512)
                    py = ps_next(0, 8)
                    for hc in range(4):
                        P.mm(py.ap, aT.ap[:, hc, blk * 128:(blk + 1) * 128], wd.ap[:, hc, cs], hc == 0, hc == 3,
                             reads=[aT, wd], writes=[py])
                    ya = yacc[tb]
                    P.stt(ya.ap[:, cs], py.ap, comb.ap[:, tb, e:e + 1], ya.ap[:, cs], ALU.mult, ALU.add,
                          reads=[py, comb, ya], writes=[ya])
                if e == NE - 1:
                    final_norm(tb)
    P.emit(final_waits=finals)
    return nc


def core_tiles(r):
    return [r, 7 - r, 8 + r, 15 - r]


def make_in_maps(inputs):
    f = lambda a: np.ascontiguousarray(np.asarray(a, dtype=np.float32))
    x = f(inputs["x"])
    consts = make_consts()
    w_rt = np.ascontiguousarray(np.concatenate([f(inputs["w_router_group"])[0], f(inputs["w_router_expert"])[0]], axis=1))
    b_rt = np.ascontiguousarray(np.concatenate([f(inputs["b_router_group"])[0], f(inputs["b_router_expert"])[0]], axis=0))
    shared = {
        "consts": consts,
        "w_in": f(inputs["w_in"])[0], "w_bsb": f(inputs["w_branch_sb"])[0], "w_bhg": f(inputs["w_branch_hg"])[0],
        "w_out": f(inputs["w_out"])[0], "w_rt": w_rt, "b_rt": b_rt,
        "w_eg": f(inputs["w_exp_gate"])[0], "w_eu": f(inputs["w_exp_up"])[0], "w_ed": f(inputs["w_exp_down"])[0],
        "ln1_g": f(inputs["ln1_g"])[0], "ln2_g": f(inputs["ln2_g"])[0], "final_g": f(inputs["final_g"]),
        "hg_norm_g": f(inputs["hg_norm_g"])[0], "hg_lb_logits": f(inputs["hg_lb_logits"]),
    }
    maps = []
    for c in range(8):
        b, r = c // 4, c % 4
        tiles = core_tiles(r)
        x_own = np.ascontiguousarray(np.concatenate([x[b, t * 512:(t + 1) * 512] for t in tiles], axis=0))
        pos = np.concatenate([np.arange(t * 512, (t + 1) * 512) for t in tiles]).astype(np.float32)
        qpos = np.ascontiguousarray(np.broadcast_to(pos[None, :], (128, 2048)))
        sel = np.zeros((64, 64), np.float32)
        for i, t in enumerate(tiles):
            sel[:, i * 16 + t] = 1.0
        m = dict(shared)
        m.update({"x_all": np.ascontiguousarray(x[b]), "x_own": x_own, "qpos": qpos, "sel": sel})
        maps.append(m)
    return maps


_NC_CACHE = {}


def kernel(**inputs):
    if "nc" not in _NC_CACHE:
        _NC_CACHE["nc"] = build_program()
    nc = _NC_CACHE["nc"]
    maps = make_in_maps(inputs)
    res = run_bass_kernel_spmd(nc, maps, core_ids=list(range(8)))
    x = np.asarray(inputs["x"])
    out = np.zeros(x.shape, np.float32)
    for c in range(8):
        b, r = c // 4, c % 4
        o = np.asarray(res.results[c]["out"])
        for i, t in enumerate(core_tiles(r)):
            out[b, t * 512:(t + 1) * 512] = o[i * 512:(i + 1) * 512]
    return out
```
